# Optimizing a Trainium2 kernel written in Bass

```python
import math
import jax, jax.numpy as jnp
from jax import lax
import numpy as np

D_MODEL = 1024
BATCH = 32
SEQ = 2048
DEPTH = 2
DEC_BATCH = 32
DEC_SEQ = 32
PAST_LEN = 1024

CHUNK = 64
N_HEADS = D_MODEL // 128
HEAD_DIM = 64
QK_DIM = 2 * HEAD_DIM
V_DIM = 2 * HEAD_DIM
QK_WIDTH = N_HEADS * QK_DIM
ATTN_WIDTH = N_HEADS * V_DIM
POOL_WINDOWS = (2, 4, 8, 16)
N_POOL_GROUPS = len(POOL_WINDOWS)
POOL_GROUP_DIM = D_MODEL // 8
POOL_WIDTH = N_POOL_GROUPS * POOL_GROUP_DIM
POOL_HIST = max(POOL_WINDOWS) - 1
IN_WIDTH = POOL_WIDTH + 2 * QK_WIDTH + ATTN_WIDTH + 2 * D_MODEL
N_BUCKETS = 32
MAX_DISTANCE = 128
Q_BLOCK = 128
D_FF = ((8 * D_MODEL // 3 + 127) // 128) * 128
N_EXPERTS = 8
TOP_K = 2
D_FF_EXPERT = 7 * D_MODEL // 2
N_DENSE = (DEPTH + 1) // 2
N_MOE = DEPTH // 2
ALPHA = (2.0 * DEPTH) ** 0.25
BETA = (8.0 * DEPTH) ** -0.25
LN_EPS = 1e-5
RMS_EPS = 1e-5

kernel_name = "chunk_causal_pool_diffattn_moe_stream"


def layer_norm(x, g, b):
    xf = x.astype(jnp.float32)
    mu = jnp.mean(xf, axis=-1, keepdims=True)
    var = jnp.mean(jnp.square(xf - mu), axis=-1, keepdims=True)
    return ((xf - mu) * lax.rsqrt(var + LN_EPS) * g + b).astype(x.dtype)


def rel_bucket(rel):
    nb = N_BUCKETS // 2
    max_exact = nb // 2
    n = jnp.abs(rel)
    large = max_exact + (jnp.log(jnp.maximum(n, 1).astype(jnp.float32) / max_exact)
                         / math.log(MAX_DISTANCE / max_exact) * (nb - max_exact)).astype(jnp.int32)
    large = jnp.minimum(large, nb - 1)
    return jnp.where(rel > 0, nb, 0) + jnp.where(n < max_exact, n, large)


def diff_attn_block(q, k, v, q_pos, k_pos, rel_bias, lam):
    rel = k_pos[None, :] - q_pos[:, None]
    bias = jnp.transpose(rel_bias[rel_bucket(rel)], (2, 0, 1)).astype(jnp.float32)
    allowed = (k_pos[None, :] // CHUNK) <= (q_pos[:, None] // CHUNK)
    logits = jnp.einsum('bqhcd,bkhcd->bchqk', q, k, preferred_element_type=jnp.float32) * (HEAD_DIM ** -0.5)
    logits = jnp.where(allowed, logits + bias, -jnp.inf)
    p = jax.nn.softmax(logits, axis=-1)
    a = p[:, 0] - lam * p[:, 1]
    return jnp.einsum('bhqk,bkhd->bqhd', a.astype(v.dtype), v)


def prompt_attention(q, k, v, pos, rel_bias, lam):
    B, S = q.shape[0], q.shape[1]
    nblk = S // Q_BLOCK
    qb = jnp.moveaxis(q.reshape(B, nblk, Q_BLOCK, N_HEADS, 2, HEAD_DIM), 1, 0)
    pb = pos.reshape(nblk, Q_BLOCK)
    out = lax.map(lambda args: diff_attn_block(args[0], k, v, args[1], pos, rel_bias, lam), (qb, pb))
    return jnp.moveaxis(out, 0, 1).reshape(B, S, N_HEADS, V_DIM)


def pool_mixer(u, hist, pos, pool_w, pool_scale):
    B, T, _ = u.shape
    full = jnp.concatenate([hist, u], axis=1)
    cs = jnp.pad(jnp.cumsum(full.astype(jnp.float32), axis=1), ((0, 0), (1, 0), (0, 0)))
    hi = cs[:, POOL_HIST + 1:POOL_HIST + 1 + T]
    means = []
    for gi, w in enumerate(POOL_WINDOWS):
        sl = slice(gi * POOL_GROUP_DIM, (gi + 1) * POOL_GROUP_DIM)
        lo = cs[:, POOL_HIST + 1 - w:POOL_HIST + 1 - w + T, sl]
        cnt = jnp.minimum(pos + 1, w).astype(jnp.float32)[None, :, None]
        means.append((hi[..., sl] - lo) / cnt)
    mean = jnp.concatenate(means, axis=-1)
    d = (mean - u.astype(jnp.float32)).astype(u.dtype).reshape(B, T, N_POOL_GROUPS, POOL_GROUP_DIM)
    out = jnp.einsum('btgc,gcd->btgd', d, pool_w).reshape(B, T, POOL_WIDTH) * pool_scale
    return out, full[:, -POOL_HIST:]


def token_mixer(h, pos, pool_hist, k_past, v_past, w_in, b_gate, pool_w, pool_scale, lam_qk, subln_g,
                w_pool_up, w_attn_up, w_o, rel_bias, layer):
    B, T, _ = h.shape
    proj = h @ w_in
    o1 = POOL_WIDTH
    o2 = o1 + QK_WIDTH
    o3 = o2 + QK_WIDTH
    o4 = o3 + ATTN_WIDTH
    u = proj[..., :o1]
    q = proj[..., o1:o2].reshape(B, T, N_HEADS, 2, HEAD_DIM)
    k = proj[..., o2:o3].reshape(B, T, N_HEADS, 2, HEAD_DIM)
    v = proj[..., o3:o4].reshape(B, T, N_HEADS, V_DIM)
    gates = jax.nn.sigmoid((proj[..., o4:] + b_gate).astype(jnp.float32)).astype(h.dtype)

    pool_out, pool_state = pool_mixer(u, pool_hist, pos, pool_w, pool_scale)

    lam_init = 0.8 - 0.6 * math.exp(-0.3 * layer)
    lf = lam_qk.astype(jnp.float32)
    lam = jnp.exp(jnp.sum(lf[0] * lf[1])) - jnp.exp(jnp.sum(lf[2] * lf[3])) + lam_init
    if k_past is None:
        o = prompt_attention(q, k, v, pos, rel_bias, lam)
    else:
        P = k_past.shape[1]
        k_all = jnp.concatenate([k_past.reshape(B, P, N_HEADS, 2, HEAD_DIM), k], axis=1)
        v_all = jnp.concatenate([v_past, v], axis=1)
        k_pos = jnp.arange(P + T, dtype=jnp.int32)
        o = diff_attn_block(q, k_all, v_all, pos, k_pos, rel_bias, lam)
    of = o.astype(jnp.float32)
    of = of * lax.rsqrt(jnp.mean(of * of, axis=-1, keepdims=True) + RMS_EPS) * subln_g * (1.0 - lam_init)
    attn_out = of.astype(h.dtype).reshape(B, T, ATTN_WIDTH)

    merged = gates[..., :D_MODEL] * (pool_out @ w_pool_up) + gates[..., D_MODEL:] * (attn_out @ w_attn_up)
    return merged @ w_o, pool_state, k.reshape(B, T, N_HEADS, QK_DIM), v


def swiglu(h, wg, wu, wd):
    return (jax.nn.silu(h @ wg) * (h @ wu)) @ wd


def moe_swiglu(h, w_router, wg, wu, wd):
    logits = (h @ w_router).astype(jnp.float32)
    top_val, top_idx = lax.top_k(logits, TOP_K)
    top_w = jax.nn.softmax(top_val, axis=-1)
    combine = jnp.sum(jax.nn.one_hot(top_idx, N_EXPERTS, dtype=jnp.float32) * top_w[..., None], axis=-2)
    combine = combine.astype(h.dtype)
    out = jnp.zeros_like(h)
    for e in range(N_EXPERTS):
        out = out + combine[..., e:e + 1] * swiglu(h, wg[e], wu[e], wd[e])
    return out


def trunk(x, c, pos, pool_hist, k_past, v_past, W):
    ks, vs, ps = [], [], []
    for l in range(DEPTH):
        mod = (jax.nn.silu(c) @ W['w_ada'][l] + W['b_ada'][l])[:, None, :]
        sh1, sc1, g1, sh2, sc2, g2 = jnp.split(mod, 6, axis=-1)
        h = x * (1 + sc1) + sh1
        mix, pst, kn, vn = token_mixer(
            h, pos, pool_hist[l],
            None if k_past is None else k_past[l], None if v_past is None else v_past[l],
            W['w_in'][l], W['b_gate'][l], W['pool_w'][l], W['pool_scale'][l], W['lam_qk'][l], W['subln_g'][l],
            W['w_pool_up'][l], W['w_attn_up'][l], W['w_o'][l], W['rel_bias'], l)
        x = layer_norm(ALPHA * x + g1 * mix, W['ln_g'][l, 0], W['ln_b'][l, 0])
        h = x * (1 + sc2) + sh2
        if l % 2 == 0:
            i = l // 2
            f = swiglu(h, W['w_ffn_gate'][i], W['w_ffn_up'][i], W['w_ffn_down'][i])
        else:
            i = l // 2
            f = moe_swiglu(h, W['w_router'][i], W['w_exp_gate'][i], W['w_exp_up'][i], W['w_exp_down'][i])
        x = layer_norm(ALPHA * x + g2 * f, W['ln_g'][l, 1], W['ln_b'][l, 1])
        ks.append(kn)
        vs.append(vn)
        ps.append(pst)
    return x, jnp.stack(ks), jnp.stack(vs), jnp.stack(ps)


def setup_inputs(seed: int = 0) -> dict:
    key = jax.random.key(seed)
    ks = jax.random.split(key, 32)
    f32 = jnp.float32
    D = D_MODEL

    def nrm(k, shape, s):
        return jax.random.normal(k, shape, f32) * s

    return {
        'x_prompt': nrm(ks[0], (BATCH, SEQ, D), 1.0),
        'x_sample': nrm(ks[1], (DEC_BATCH, DEC_SEQ, D), 1.0),
        'cache_k': nrm(ks[2], (DEPTH, DEC_BATCH, PAST_LEN, N_HEADS, QK_DIM), 1.0),
        'cache_v': nrm(ks[3], (DEPTH, DEC_BATCH, PAST_LEN, N_HEADS, V_DIM), 1.0),
        'state_pool': nrm(ks[4], (DEPTH, DEC_BATCH, POOL_HIST, POOL_WIDTH), 1.0),
        'c_prompt': nrm(ks[5], (BATCH, D), 1.0),
        'c_sample': nrm(ks[6], (DEC_BATCH, D), 1.0),
        'rel_bias': nrm(ks[7], (N_BUCKETS, N_HEADS), 0.5),
        'w_ada': nrm(ks[8], (DEPTH, D, 6 * D), 0.5 * D ** -0.5),
        'b_ada': nrm(ks[9], (DEPTH, 6 * D), 0.02),
        'w_in': nrm(ks[10], (DEPTH, D, IN_WIDTH), D ** -0.5),
        'b_gate': nrm(ks[11], (DEPTH, 2 * D), 0.02),
        'pool_w': nrm(ks[12], (DEPTH, N_POOL_GROUPS, POOL_GROUP_DIM, POOL_GROUP_DIM), POOL_GROUP_DIM ** -0.5),
        'pool_scale': 1.0 + nrm(ks[13], (DEPTH, POOL_WIDTH), 0.02),
        'lam_qk': nrm(ks[14], (DEPTH, 4, HEAD_DIM), 0.1),
        'subln_g': 1.0 + nrm(ks[15], (DEPTH, V_DIM), 0.02),
        'w_pool_up': nrm(ks[16], (DEPTH, POOL_WIDTH, D), POOL_WIDTH ** -0.5),
        'w_attn_up': nrm(ks[17], (DEPTH, ATTN_WIDTH, D), ATTN_WIDTH ** -0.5),
        'w_o': nrm(ks[18], (DEPTH, D, D), BETA * D ** -0.5),
        'ln_g': 1.0 + nrm(ks[19], (DEPTH, 2, D), 0.02),
        'ln_b': nrm(ks[20], (DEPTH, 2, D), 0.02),
        'w_ffn_gate': nrm(ks[21], (N_DENSE, D, D_FF), D ** -0.5),
        'w_ffn_up': nrm(ks[22], (N_DENSE, D, D_FF), D ** -0.5),
        'w_ffn_down': nrm(ks[23], (N_DENSE, D_FF, D), BETA * D_FF ** -0.5),
        'w_router': nrm(ks[24], (N_MOE, D, N_EXPERTS), D ** -0.5),
        'w_exp_gate': nrm(ks[25], (N_MOE, N_EXPERTS, D, D_FF_EXPERT), D ** -0.5),
        'w_exp_up': nrm(ks[26], (N_MOE, N_EXPERTS, D, D_FF_EXPERT), D ** -0.5),
        'w_exp_down': nrm(ks[27], (N_MOE, N_EXPERTS, D_FF_EXPERT, D), BETA * D_FF_EXPERT ** -0.5),
    }


def reference(x_prompt, x_sample, cache_k, cache_v, state_pool, c_prompt, c_sample, rel_bias, w_ada, b_ada,
              w_in, b_gate, pool_w, pool_scale, lam_qk, subln_g, w_pool_up, w_attn_up, w_o, ln_g, ln_b,
              w_ffn_gate, w_ffn_up, w_ffn_down, w_router, w_exp_gate, w_exp_up, w_exp_down):
    W = dict(rel_bias=rel_bias, w_ada=w_ada, b_ada=b_ada, w_in=w_in, b_gate=b_gate, pool_w=pool_w,
             pool_scale=pool_scale, lam_qk=lam_qk, subln_g=subln_g, w_pool_up=w_pool_up, w_attn_up=w_attn_up,
             w_o=w_o, ln_g=ln_g, ln_b=ln_b, w_ffn_gate=w_ffn_gate, w_ffn_up=w_ffn_up, w_ffn_down=w_ffn_down,
             w_router=w_router, w_exp_gate=w_exp_gate, w_exp_up=w_exp_up, w_exp_down=w_exp_down)

    B, S = x_prompt.shape[0], x_prompt.shape[1]
    pos_p = jnp.arange(S, dtype=jnp.int32)
    pool_zero = jnp.zeros((DEPTH, B, POOL_HIST, POOL_WIDTH), x_prompt.dtype)
    y_prompt, new_k_prompt, new_v_prompt, new_pool_prompt = trunk(
        x_prompt, c_prompt, pos_p, pool_zero, None, None, W)

    P = cache_k.shape[2]
    T = x_sample.shape[1]
    pos_s = P + jnp.arange(T, dtype=jnp.int32)
    y_sample, new_k_sample, new_v_sample, new_pool_sample = trunk(
        x_sample, c_sample, pos_s, state_pool, cache_k, cache_v, W)

    return (y_prompt, y_sample, new_k_prompt, new_v_prompt, new_pool_prompt,
            new_k_sample, new_v_sample, new_pool_sample)
```

```python
import math
import os
import numpy as np
import concourse.bass as bass
import concourse.mybir as mybir
from concourse.bass_utils import run_bass_kernel_spmd

F32 = mybir.dt.float32
BF16 = mybir.dt.bfloat16
AF = mybir.ActivationFunctionType
ALU = mybir.AluOpType
AX = mybir.AxisListType

D = 1024
NKC = 8
NH = 8
DEPTH = 2
SEQ = 2048
DEC_SEQ = 32
PAST = 1024
IN_W = 5632
O_Q, O_K, O_V, O_GP, O_GA = 512, 1536, 2560, 3584, 4608
DFF = 2816
DFE = 3584
NE = 8
ALPHA = (2.0 * DEPTH) ** 0.25
LN_EPS = 1e-5
RMS_EPS = 1e-5
POOL_W = (2, 4, 8, 16)
LAM_INIT = [0.8 - 0.6 * math.exp(-0.3 * l) for l in range(DEPTH)]
N_BUCKETS = 32
MAX_DISTANCE = 128
NCORES = 8

EPOCH = 16384
SAME_ENGINE_SYNC = True


class Res:
    __slots__ = ("name", "last_w", "reads", "excl")

    def __init__(self, name, excl=False):
        self.name = name
        self.last_w = None
        self.reads = []
        self.excl = excl


class Eng:
    def __init__(self, fw, name, is_pe=False):
        self.fw = fw
        self.name = name
        self.is_pe = is_pe
        self.count = 0
        self.sems = []
        self.known = {}
        self.dknown = {}
        self.prog = []

    def sem_for(self, n):
        e = (n - 1) // EPOCH
        while len(self.sems) <= e:
            self.sems.append(self.fw.nc.alloc_semaphore(f"s_{self.name}_{len(self.sems)}"))
        return self.sems[e], (n - 1) % EPOCH + 1


class FW:
    def __init__(self, nc):
        self.nc = nc
        self.dry = False
        self.pe = Eng(self, "pe", True)
        self.act = Eng(self, "act")
        self.dve = Eng(self, "dve")
        self.pool = Eng(self, "pool")
        self.sp = Eng(self, "sp")
        self.engs = {e.name: e for e in (self.pe, self.act, self.dve, self.pool, self.sp)}
        self.dma_sems = {}
        self.n_wait = 0
        self.n_ins = 0

    def _wait(self, eng, ev):
        if ev is None:
            return
        if ev[0] == "e":
            _, ename, n = ev
            if ename == eng.name and (eng.is_pe or not SAME_ENGINE_SYNC):
                return
            if eng.known.get(ename, 0) >= n:
                return
            sem, val = self.engs[ename].sem_for(n)
            self._emit_wait(eng, sem, val)
            eng.known[ename] = n
        else:
            _, key, val = ev
            if eng.dknown.get(key, 0) >= val:
                return
            self._emit_wait(eng, self.dma_sems[key][0], val)
            eng.dknown[key] = val

    def _emit_wait(self, eng, sem, val):
        i = len(eng.prog) - 1
        while i >= 0 and eng.prog[i][0] == "w":
            if eng.prog[i][1] is sem or eng.prog[i][1] == sem:
                if eng.prog[i][2] < val:
                    eng.prog[i] = ("w", sem, val)
                return
            i -= 1
        eng.prog.append(("w", sem, val))
        self.n_wait += 1

    def _deps(self, eng, reads, writes):
        need = {}
        def add(ev):
            if ev is None:
                return
            k = (ev[0], ev[1])
            if k not in need or need[k][2] < ev[2]:
                need[k] = ev
        for r in reads:
            add(r.last_w)
        for w in writes:
            add(w.last_w)
            for ev in w.reads:
                add(ev)
        for ev in need.values():
            self._wait(eng, ev)

    def _record(self, ev, reads, writes):
        for r in reads:
            r.reads.append(ev)
            if len(r.reads) > 40:
                last = {}
                for e in r.reads:
                    last[(e[0], e[1])] = e
                r.reads = list(last.values())
        for w in writes:
            w.last_w = ev
            w.reads = []

    def op(self, eng, fn, reads=(), writes=()):
        if self.dry:
            return
        ex = [r for r in reads if r.excl]
        if ex:
            writes = list(writes) + ex
            reads = [r for r in reads if not r.excl]
        self._deps(eng, reads, writes)
        eng.count += 1
        sem, _ = eng.sem_for(eng.count)
        eng.prog.append(("i", fn, sem, 1))
        self.n_ins += 1
        self._record(("e", eng.name, eng.count), reads, writes)

    def mm_group(self, fns, reads=(), writes=()):
        if self.dry:
            return
        eng = self.pe
        self._deps(eng, reads, writes)
        for fn in fns[:-1]:
            eng.prog.append(("i", fn, None, 0))
        eng.count += 1
        sem, _ = eng.sem_for(eng.count)
        eng.prog.append(("i", fns[-1], sem, 1))
        self.n_ins += len(fns)
        self._record(("e", eng.name, eng.count), reads, writes)

    def dma(self, q, key, out, in_, reads=(), writes=()):
        if self.dry:
            return
        self._deps(q, reads, writes)
        if key not in self.dma_sems:
            self.dma_sems[key] = [self.nc.alloc_semaphore(f"d_{key}"), 0]
        ent = self.dma_sems[key]
        ent[1] += 16
        assert ent[1] < 60000, key
        qn = q.name
        q.prog.append(("d", out, in_, ent[0], 16))
        self.n_ins += 1
        self._record(("d", key, ent[1]), reads, writes)

    def barrier(self, engines=("pe", "act", "dve", "sp")):
        if self.dry:
            return
        for en in engines:
            eng = self.engs[en]
            for other in self.engs.values():
                if other.name == en or other.count == 0:
                    continue
                self._wait(eng, ("e", other.name, other.count))
            for key, ent in self.dma_sems.items():
                if ent[1] > 0:
                    self._wait(eng, ("d", key, ent[1]))

    def dump(self, path):
        with open(path, "w") as f:
            for en, eng in self.engs.items():
                f.write(f"==== {en}\n")
                for i, item in enumerate(eng.prog):
                    if item[0] == "w":
                        f.write(f"{i} WAIT {item[1]} >= {item[2]}\n")
                    elif item[0] == "i":
                        f.write(f"{i} INS inc={item[2]} {item[3]} {getattr(item[1], '_dbg', '')}\n")
                    else:
                        f.write(f"{i} DMA inc={item[3]}\n")

    def flush(self):
        nc = self.nc
        import os
        if os.environ.get("K_DUMP"):
            self.dump(os.environ["K_DUMP"])
        hmap = {"sp": "sync", "pool": "gpsimd", "act": "scalar", "dve": "vector", "pe": "tensor"}
        with nc.Block() as block:
            def run(eng):
                def body(e):
                    for item in eng.prog:
                        k = item[0]
                        if k == "w":
                            e.wait_ge(item[1], item[2])
                        elif k == "i":
                            ins = item[1]()
                            if item[2] is not None:
                                ins.then_inc(item[2], item[3])
                        else:
                            e.dma_start(out=item[1], in_=item[2]).then_inc(item[3], item[4])
                return body
            for en, bn in hmap.items():
                getattr(block, bn)(run(self.engs[en]))


class WStream:
    NSLOT = 8
    LOOK = 4

    def __init__(self, fw, nc):
        self.fw = fw
        self.slots = [nc.alloc_sbuf_tensor(f"wslot{i}", [128, 8, 128], BF16) for i in range(self.NSLOT)]
        self.res = [Res(f"wslot{i}") for i in range(self.NSLOT)]
        self.reqs = []
        self.n_issued = 0
        self.n_cons = 0
        self.released = 0

    def reset_for_real(self):
        self.n_issued = 0
        self.n_cons = 0
        self.released = 0

    def _issue(self):
        fw = self.fw
        while (self.n_issued < len(self.reqs) and self.n_issued <= self.n_cons + self.LOOK
               and self.n_issued - self.NSLOT < self.released):
            i = self.n_issued
            s = i % self.NSLOT
            src, n = self.reqs[i]
            fw.dma(fw.pool, f"w{s}", self.slots[s][:, 0:n, :], src, writes=[self.res[s]])
            self.n_issued += 1

    def get(self, src, n):
        if self.fw.dry:
            self.reqs.append((src, n))
            return self.slots[0], self.res[0]
        idx = self.n_cons
        self._issue()
        assert self.n_issued > idx, "weight stream discipline violated"
        self.n_cons += 1
        self._issue()
        s = idx % self.NSLOT
        return self.slots[s], self.res[s]

    def free(self):
        if self.fw.dry:
            return
        self.released = self.n_cons
        self._issue()


def wblock(w2d, r0, n, c0):
    return w2d[r0:r0 + 128 * n, c0:c0 + 128].rearrange("(i p) c -> p i c", p=128)


def _bucket(rel):
    nb = N_BUCKETS // 2
    max_exact = nb // 2
    n = np.abs(rel)
    large = max_exact + (np.log(np.maximum(n, 1).astype(np.float32) / max_exact)
                         / math.log(MAX_DISTANCE / max_exact) * (nb - max_exact)).astype(np.int32)
    large = np.minimum(large, nb - 1)
    return np.where(rel > 0, nb, 0) + np.where(n < max_exact, n, large)


def _bucket_onehot():
    m = np.arange(768)
    b = _bucket(127 - m)
    oh = np.zeros((32, 768), np.float32)
    oh[b, m] = 1.0
    return oh


class Builder:
    def __init__(self, n_seq, T, with_sample):
        self.n_seq, self.T, self.with_sample = n_seq, T, with_sample
        nc = self.nc = bass.Bass("TRN2", target_bir_lowering=False)
        self.fw = FW(nc)
        self._declare_io()
        self._alloc()

    def _declare_io(self):
        nc, n_seq, T = self.nc, self.n_seq, self.T

        def din(name, shape):
            return nc.dram_tensor(name, list(shape), F32, kind="ExternalInput").ap()

        def dout(name, shape):
            return nc.dram_tensor(name, list(shape), F32, kind="ExternalOutput").ap()

        self.xp = din("xp", [n_seq, 8, 128, T])
        self.xs = din("xs", [8, 128, 128])
        self.ckT = din("ckT", [2, 4, 8, 128, PAST])
        self.cv = din("cv", [2, 4, PAST, 8, 128])
        self.spT = din("spT", [2, 4, 4, 128, 15])
        self.cT = din("cT", [128, 64])
        self.relb = din("relb", [32, 8])
        self.ohm = din("ohm", [32, 768])
        self.w_ada = din("w_ada", [2, D, 6 * D])
        self.b_ada = din("b_ada_t", [128, 2, 48])
        self.w_in = din("w_in", [2, D, IN_W])
        self.b_gate = din("b_gate_t", [128, 2, 16])
        self.pool_w = din("pool_w", [2, 4, 128, 128])
        self.pscale = din("pscale_t", [128, 2, 4])
        self.lamqk = din("lamqk_b", [128, 2, 256])
        self.subln = din("subln_t", [128, 2])
        self.w_pool_up = din("w_pool_up", [2, 512, D])
        self.w_attn_up = din("w_attn_up", [2, D, D])
        self.w_o = din("w_o", [2, D, D])
        self.lng = din("lng_t", [128, 2, 2, 8])
        self.lnb = din("lnb_t", [128, 2, 2, 8])
        self.wfg = din("w_ffn_gate", [1, D, DFF])
        self.wfu = din("w_ffn_up", [1, D, DFF])
        self.wfd = din("w_ffn_down", [1, DFF, D])
        self.w_router = din("w_router_t", [128, 8, 8])
        self.weg = din("w_exp_gate", [1, NE, D, DFE])
        self.weu = din("w_exp_up", [1, NE, D, DFE])
        self.wed = din("w_exp_down", [1, NE, DFE, D])
        self.yp = dout("yp", [n_seq, T, D])
        self.ys = dout("ys", [128, D])
        self.nkp = dout("nkp", [2, n_seq, T, 8, 128])
        self.nvp = dout("nvp", [2, n_seq, T, 8, 128])
        self.npp = dout("npp", [2, n_seq, 15, 512])
        self.nks = dout("nks", [2, 4, 32, 8, 128])
        self.nvs = dout("nvs", [2, 4, 32, 8, 128])
        self.nps = dout("nps", [2, 4, 15, 512])
        self.gscr = nc.dram_tensor("gscr", [8, 128 * 768], F32, kind="Internal")
        self.wscr = nc.dram_tensor("wscr", [8, 128, 640], F32, kind="Internal")

    def _alloc(self):
        nc, T = self.nc, self.T
        sb = nc.alloc_sbuf_tensor
        self.X = sb("X", [128, 8, T], F32)
        self.Hh = sb("H", [128, 8, T], BF16)
        self.AO = sb("AO", [128, 8, T], BF16)
        self.PO = sb("PO", [128, 4, T], BF16)
        self.FA = sb("FA", [128, 4608], F32)
        self.BA = sb("BA", [128, 8192], BF16)
        self.RX = [Res(f"X{k}") for k in range(8)]
        self.RH = [Res(f"H{k}") for k in range(8)]
        self.RAO = [Res(f"AO{k}") for k in range(8)]
        self.RPO = [Res(f"PO{k}") for k in range(4)]
        self.ps = [nc.alloc_psum_tensor(f"ps{i}", [128, 512], F32) for i in range(8)]
        self.RP = [Res(f"ps{i}", excl=True) for i in range(8)]
        self.bank_rr = 0
        self.ones_mean = sb("ones_mean", [128, 128], F32)
        self.ones_rms = sb("ones_rms", [128, 128], F32)
        self.ones_f = sb("ones_f", [128, 128], F32)
        self.ones_b = sb("ones_b", [128, 128], BF16)
        self.ident = sb("ident", [128, 128], F32)
        self.eps = sb("eps", [128, 2], F32)
        self.t_bada = sb("t_bada", [128, 2, 48], F32)
        self.t_bgate = sb("t_bgate", [128, 2, 16], F32)
        self.t_pscale = sb("t_pscale", [128, 2, 4], F32)
        self.t_subln = sb("t_subln", [128, 2], F32)
        self.t_lng = sb("t_lng", [128, 2, 2, 8], F32)
        self.t_lnb = sb("t_lnb", [128, 2, 2, 8], F32)
        self.t_relb = sb("t_relb", [128, 8], F32)
        self.t_router = sb("t_router", [128, 8, 8], F32)
        self.MOD = sb("MOD", [128, 2, 48, 8], F32)
        self.SCA = sb("SCA", [128, 2, 2, 8, 8], F32)
        self.SCB = sb("SCB", [128, 2, 2, 8, 8], F32)
        self.LAMT = sb("LAMT", [128, 2, 8], F32)
        self.BF15 = sb("BF15", [128, 8], F32)
        self.INVC = sb("INVC", [128, 4, 16], F32)
        self.WH = [sb(f"WH{i}", [128, 640], F32) for i in range(2)]
        self.RWH = [Res(f"WH{i}") for i in range(2)]
        self.EX = [self.FA[:, 1024:1536], self.FA[:, 1536:2048]]
        self.REX = [Res(f"EX{i}") for i in range(2)]
        self.RC = Res("consts")
        self.ROUT = Res("outputs")
        self.RSCR = Res("dram_scratch")
        self.wst = WStream(self.fw, nc)

    def bank(self, allowed=None):
        allowed = allowed or range(8)
        allowed = list(allowed)
        i = allowed[self.bank_rr % len(allowed)]
        self.bank_rr += 1
        return i

    def mm(self, out, lhsT, rhs, start, stop):
        nc = self.nc
        return lambda: nc.tensor.matmul(out, lhsT, rhs, start=start, stop=stop)

    def tr(self, out, in_):
        nc = self.nc
        ident = self.ident
        return lambda: nc.tensor.transpose(out, in_, ident[:])

    def mm_acc(self, out, pairs, reads, writes):
        n = len(pairs)
        fns = [self.mm(out, l, r, i == 0, i == n - 1) for i, (l, r) in enumerate(pairs)]
        self.fw.mm_group(fns, reads, writes)

    def act(self, out, in_, func, reads, writes, bias=None, scale=None):
        nc = self.nc
        kw = {}
        if bias is not None:
            kw["bias"] = bias
        if scale is not None:
            kw["scale"] = scale
        self.fw.op(self.fw.act, lambda: nc.scalar.activation(out=out, in_=in_, func=func, **kw), reads, writes)

    def acopy(self, out, in_, reads, writes):
        nc = self.nc
        self.fw.op(self.fw.act, lambda: nc.scalar.copy(out, in_), reads, writes)

    def vcopy(self, out, in_, reads, writes):
        nc = self.nc
        self.fw.op(self.fw.dve, lambda: nc.vector.tensor_copy(out, in_), reads, writes)

    def vtt(self, out, in0, in1, op, reads, writes):
        nc = self.nc
        self.fw.op(self.fw.dve, lambda: nc.vector.tensor_tensor(out=out, in0=in0, in1=in1, op=op), reads, writes)

    def vts(self, out, in_, scalar, op, reads, writes):
        nc = self.nc
        self.fw.op(self.fw.dve, lambda: nc.vector.tensor_single_scalar(out=out, in_=in_, scalar=scalar, op=op),
                   reads, writes)

    def vts2(self, out, in_, s1, s2, op0, op1, reads, writes):
        nc = self.nc
        self.fw.op(self.fw.dve, lambda: nc.vector.tensor_scalar(out=out, in0=in_, scalar1=s1, scalar2=s2,
                                                                op0=op0, op1=op1), reads, writes)

    def vstt(self, out, in0, scalar, in1, op0, op1, reads, writes):
        nc = self.nc
        self.fw.op(self.fw.dve, lambda: nc.vector.scalar_tensor_tensor(out=out, in0=in0, scalar=scalar, in1=in1,
                                                                       op0=op0, op1=op1), reads, writes)

    def vmemset(self, ap, val, writes):
        nc = self.nc
        self.fw.op(self.fw.dve, lambda: nc.vector.memset(ap, val), (), writes)

    def dma_sp(self, key, out, in_, reads, writes):
        self.fw.dma(self.fw.sp, key, out, in_, reads, writes)

    def setup(self):
        nc, fw = self.nc, self.fw
        RC = self.RC
        FA = self.FA
        t_lamqk = FA[:, 128:640]
        self.vmemset(self.t_relb[:], 0.0, [RC])
        loads = [(self.t_bada, self.b_ada), (self.t_bgate, self.b_gate), (self.t_pscale, self.pscale),
                 (t_lamqk, self.lamqk.rearrange("p l n -> p (l n)")), (self.t_subln, self.subln),
                 (self.t_lng, self.lng), (self.t_lnb, self.lnb), (self.t_relb[0:32, :], self.relb),
                 (FA[:, 0:64], self.cT), (self.t_router, self.w_router)]
        for i, (t, src) in enumerate(loads):
            self.dma_sp(f"ld{i}", t[:] if not isinstance(t, bass.AP) else t, src, (), [RC])
        self.vmemset(self.ones_mean[:], 1.0 / 1024, [RC])
        self.vmemset(self.ones_rms[:], 1.0 / 128, [RC])
        self.vmemset(self.ones_f[:], 1.0, [RC])
        self.vmemset(self.ones_b[:], 1.0, [RC])
        self.vmemset(self.eps[:, 0:1], LN_EPS, [RC])
        self.vmemset(self.eps[:, 1:2], RMS_EPS, [RC])
        ident, INVC = self.ident, self.INVC
        fw.op(fw.pool, lambda: nc.gpsimd.memset(ident[:], 1.0), (), [RC])
        fw.op(fw.pool, lambda: nc.gpsimd.affine_select(out=ident[:], in_=ident[:], pattern=[[-1, 128]],
                                                       compare_op=ALU.is_equal, fill=0.0, base=0,
                                                       channel_multiplier=1), [RC], [RC])
        for g, w in enumerate(POOL_W):
            for q in range(16):
                self.vmemset(INVC[:, g, q:q + 1], 1.0 / min(q + 1, w), [RC])
        self.tick("s_consts")
        L = self.LAMT
        for l in range(2):
            tmp = FA[:, 640:704]
            RT = Res("tmp")
            for i in range(2):
                o = 256 * l + 128 * i
                self.vtt(tmp, t_lamqk[:, o:o + 64], t_lamqk[:, o + 64:o + 128], ALU.mult, [RC], [RT])
                fw.op(fw.dve, (lambda i=i, l=l: nc.vector.reduce_sum(out=L[:, l, 2 + i:3 + i], in_=tmp, axis=AX.X)),
                      [RT], [RC])
            self.act(L[:, l, 2:4], L[:, l, 2:4], AF.Exp, [RC], [RC])
            self.vtt(L[:, l, 0:1], L[:, l, 3:4], L[:, l, 2:3], ALU.subtract, [RC], [RC])
            self.vts(L[:, l, 0:1], L[:, l, 0:1], -LAM_INIT[l], ALU.add, [RC], [RC])
            self.vts(L[:, l, 1:2], self.t_subln[:, l:l + 1], 1.0 - LAM_INIT[l], ALU.mult, [RC], [RC])
        self.tick("s_lam")
        fw.barrier()
        scT = FA[:, 64:128]
        RS = Res("scT")
        self.act(scT, FA[:, 0:64], AF.Silu, [RC], [RS])
        wa = [FA[:, 512 + 1024 * i: 1536 + 1024 * i] for i in range(4)]
        RW = [Res(f"wa{i}") for i in range(4)]
        cnt = 0
        for l in range(2):
            for j in range(48):
                s = cnt % 4
                cnt += 1
                self.dma_sp(f"wa{s}", wa[s].rearrange("p (k c) -> p k c", k=8),
                            wblock(self.w_ada[l], 0, 8, j * 128), (), [RW[s]])
                b = self.bank()
                self.mm_acc(self.ps[b][:, 0:8],
                            [(wa[s][:, kc * 128:(kc + 1) * 128], scT[:, kc * 8:(kc + 1) * 8]) for kc in range(8)],
                            [RW[s], RS], [self.RP[b]])
                self.vts(self.MOD[:, l, j, :], self.ps[b][:, 0:8], self.t_bada[:, l, j:j + 1], ALU.add,
                         [self.RP[b], RC], [RC])
        self.tick("s_ada")
        M, SCA, SCB = self.MOD, self.SCA, self.SCB
        for l in range(2):
            for which in range(2):
                sh0 = 0 if which == 0 else 24
                sc0 = 8 if which == 0 else 32
                for kc in range(8):
                    a = SCA[:, l, which, kc, :]
                    bb = SCB[:, l, which, kc, :]
                    self.vts(a, M[:, l, sc0 + kc, :], 1.0, ALU.add, [RC], [RC])
                    if which == 0 and l == 0:
                        self.vcopy(bb, M[:, l, sh0 + kc, :], [RC], [RC])
                        continue
                    ll, wi = (l, 0) if which == 1 else (l - 1, 1)
                    g_ap = self.t_lng[:, ll, wi, kc:kc + 1]
                    b_ap = self.t_lnb[:, ll, wi, kc:kc + 1]
                    self.vstt(bb, a, b_ap, M[:, l, sh0 + kc, :], ALU.mult, ALU.add, [RC], [RC])
                    self.vts(a, a, g_ap, ALU.mult, [RC], [RC])
        self.tick("s_modtab")
        fw.barrier()
        RB = FA[:, 128:256]
        RRB = Res("RB")
        GT = FA[:, 256:1024]
        RG = Res("GT")
        WR = FA[:, 1024:1664]
        ALM = FA[:, 1664:2304]
        t_ohm = FA[:, 2304:3072]
        WRb = self.BA[:, 0:640]
        RWR = Res("WR")
        self.vmemset(t_ohm, 0.0, [RC])
        self.dma_sp("ld_ohm", FA[0:32, 2304:3072], self.ohm, (), [RC])
        self.vmemset(ALM, 1.0, [RC])
        self.vmemset(FA[64:128, 1664:1728], 0.0, [RC])
        self.tick("s_b0")
        import os
        for h in [int(c) for c in os.environ.get("K_HEADS", "01234567")]:
            if os.environ.get("K_E3"):
                self._e3 = getattr(self, "_e3", 0) + 1
                if self._e3 % 2 == 0:
                    GT = FA[:, 3072:3840]
                    RG = Res("GT2")
            it2 = getattr(self, "_it", 0)
            self._it = it2 + 1
            sub = int(os.environ.get("K_SUB", "99")) if it2 >= 1 else 99
            self.vts(RB, self.ones_f[:], self.t_relb[:, h:h + 1], ALU.mult, [RC], [RRB])
            if sub <= 1: raise StopIteration
            b0, b1 = self.bank(), self.bank()
            self.fw.mm_group([self.mm(self.ps[b0][:, 0:384], RB, t_ohm[:, 0:384], True, True),
                              self.mm(self.ps[b1][:, 0:384], RB, t_ohm[:, 384:768], True, True)],
                             [RRB, RC], [self.RP[b0], self.RP[b1]])
            if sub <= 2: raise StopIteration
            self.vcopy(self.BF15[:, h:h + 1], self.ps[b0][:, 300:301], [self.RP[b0]], [RC])
            if sub <= 3: raise StopIteration
            self.act(GT[:, 0:384], self.ps[b0][:, 0:384], AF.Exp, [self.RP[b0]], [RG])
            if sub <= 4: raise StopIteration
            self.act(GT[:, 384:768], self.ps[b1][:, 0:384], AF.Exp, [self.RP[b1]], [RG])
            if h <= 1: self.tick("s_b1")
            self.dma_sp("gscr", self.gscr.ap()[h].rearrange("(p n) -> p n", p=128), GT, [RG], [self.RSCR])
            src = bass.AP(tensor=self.gscr, offset=h * 128 * 768 + 127, ap=[[767, 128], [1, 640]])
            if h <= 1: self.tick("s_b2")
            self.dma_sp("toe", WR, src, [self.RSCR], [RWR])
            if h <= 1: self.tick("s_b3")
            self.vtt(WR, WR, ALM, ALU.mult, [RWR, RC], [RWR])
            if h <= 1: self.tick("s_b4")
            self.dma_sp("wscr", self.wscr.ap()[h], WR, [RWR], [self.RSCR])
            if h <= 1: self.tick("s_b5")
        fw.barrier()

    def make_group(self, kind, s):
        T = self.T if kind == "p" else 128
        TS = min(512, T)
        g = dict(kind=kind, s=s, T=T, TS=TS, NT=T // TS)
        if kind == "p":
            g["segs"] = [(0, T, s)]
        else:
            g["segs"] = [(32 * b, 32, 4 + b) for b in range(4)]
        return g

    def segs_in(self, g, t):
        TS = g["TS"]
        lo, hi = t * TS, (t + 1) * TS
        out = []
        for (s0, sl, bc) in g["segs"]:
            a, b = max(lo, s0), min(hi, s0 + sl)
            if a < b:
                out.append((a, b, bc))
        return out

    def modulate0(self, g):
        X, Hh = self.X, self.Hh
        for t in range(g["NT"]):
            for kc in range(8):
                for (a, b, bc) in self.segs_in(g, t):
                    self.act(Hh[:, kc, a:b], X[:, kc, a:b], AF.Identity, [self.RX[kc], self.RC], [self.RH[kc]],
                             bias=self.SCB[:, 0, 0, kc, bc:bc + 1], scale=self.SCA[:, 0, 0, kc, bc:bc + 1])

    def layernorm(self, g, l, which):
        fw = self.fw
        X, Hh, FA = self.X, self.Hh, self.FA
        TS = g["TS"]
        last = (l == 1 and which == 1)
        router = (l == 1 and which == 0)
        nl, nw = (l, 1) if which == 0 else (l + 1, 0)
        SQ = [FA[:, 0:512], FA[:, 512:1024]]
        RSQ = [Res("sq0"), Res("sq1")]
        MEAN = FA[:, 1024:1536]
        RSTD = FA[:, 1536:2048]
        TMP = [FA[:, 2048:2560], FA[:, 2560:3072]]
        HF = [FA[:, 3072:3584], FA[:, 3584:4096]]
        RM, RR = Res("mean"), Res("rstd")
        RT = [Res("t0"), Res("t1")]
        RHF = [Res("hf0"), Res("hf1")]
        for t in range(g["NT"]):
            c0, c1 = t * TS, (t + 1) * TS
            b1, b2 = self.bank(), self.bank()
            for kc in range(8):
                s = kc % 2
                self.vtt(SQ[s][:, 0:TS], X[:, kc, c0:c1], X[:, kc, c0:c1], ALU.mult, [self.RX[kc]], [RSQ[s]])
                self.fw.mm_group([self.mm(self.ps[b1][:, 0:TS], self.ones_mean[:], X[:, kc, c0:c1], kc == 0, kc == 7),
                                  self.mm(self.ps[b2][:, 0:TS], self.ones_mean[:], SQ[s][:, 0:TS], kc == 0, kc == 7)],
                                 [self.RX[kc], RSQ[s], self.RC], [self.RP[b1], self.RP[b2]])
            self.vcopy(MEAN[:, 0:TS], self.ps[b1][:, 0:TS], [self.RP[b1]], [RM])
            self.vtt(RSTD[:, 0:TS], MEAN[:, 0:TS], MEAN[:, 0:TS], ALU.mult, [RM], [RR])
            self.vtt(RSTD[:, 0:TS], self.ps[b2][:, 0:TS], RSTD[:, 0:TS], ALU.subtract, [self.RP[b2], RR], [RR])
            self.act(RSTD[:, 0:TS], RSTD[:, 0:TS], AF.Ln, [RR, self.RC], [RR], bias=self.eps[:, 0:1], scale=1.0)
            self.act(RSTD[:, 0:TS], RSTD[:, 0:TS], AF.Exp, [RR], [RR], scale=-0.5)
            if router:
                lb = self.bank()
            for kc in range(8):
                s = kc % 2
                self.vtt(TMP[s][:, 0:TS], X[:, kc, c0:c1], MEAN[:, 0:TS], ALU.subtract, [self.RX[kc], RM], [RT[s]])
                self.vtt(TMP[s][:, 0:TS], TMP[s][:, 0:TS], RSTD[:, 0:TS], ALU.mult, [RT[s], RR], [RT[s]])
                for (a, b, bc) in self.segs_in(g, t):
                    la, lb_ = a - c0, b - c0
                    self.act(X[:, kc, a:b], TMP[s][:, la:lb_], AF.Identity, [RT[s], self.RC], [self.RX[kc]],
                             bias=self.t_lnb[:, l, which, kc:kc + 1], scale=self.t_lng[:, l, which, kc:kc + 1])
                    if not last:
                        self.act(Hh[:, kc, a:b], TMP[s][:, la:lb_], AF.Identity, [RT[s], self.RC], [self.RH[kc]],
                                 bias=self.SCB[:, nl, nw, kc, bc:bc + 1], scale=self.SCA[:, nl, nw, kc, bc:bc + 1])
                    if router:
                        self.act(HF[s][:, la:lb_], TMP[s][:, la:lb_], AF.Identity, [RT[s], self.RC], [RHF[s]],
                                 bias=self.SCB[:, nl, nw, kc, bc:bc + 1], scale=self.SCA[:, nl, nw, kc, bc:bc + 1])
                if router:
                    nb = TS // 128
                    self.fw.mm_group([self.mm(self.ps[lb][:, 32 * kc + 8 * q:32 * kc + 8 * q + 8],
                                              HF[s][:, 128 * q:128 * q + 128], self.t_router[:, kc, :], True, True)
                                      for q in range(nb)],
                                     [RHF[s], self.RC], [self.RP[lb]])
            if router:
                self.route(g, t, lb)

    def route(self, g, t, lb):
        FA = self.FA
        TS = g["TS"]
        nb = TS // 128
        R = Res("route")
        LG = FA[:, 4096:4096 + 32]
        M1 = FA[:, 4128:4132]
        M2 = FA[:, 4132:4136]
        K1 = FA[:, 4136:4168]
        K2 = FA[:, 4168:4200]
        L2 = FA[:, 4200:4232]
        W1 = FA[:, 4232:4236]
        W2 = FA[:, 4236:4240]
        CB = FA[:, 4480 + t * nb * 8: 4480 + (t + 1) * nb * 8]
        nc, fw = self.nc, self.fw
        self.vcopy(LG[:, 0:8 * nb], self.ps[lb][:, 0:8 * nb], [self.RP[lb]], [R])
        for kc in range(1, 8):
            self.vtt(LG[:, 0:8 * nb], LG[:, 0:8 * nb], self.ps[lb][:, 32 * kc:32 * kc + 8 * nb], ALU.add,
                     [self.RP[lb], R], [R])
        for q in range(nb):
            lg = LG[:, 8 * q:8 * q + 8]
            fw.op(fw.dve, (lambda q=q, lg=lg: nc.vector.reduce_max(out=M1[:, q:q + 1], in_=lg, axis=AX.X)), [R], [R])
            self.vts(K1[:, 8 * q:8 * q + 8], lg, M1[:, q:q + 1], ALU.is_equal, [R], [R])
            self.vstt(L2[:, 8 * q:8 * q + 8], K1[:, 8 * q:8 * q + 8], -1e30, lg, ALU.mult, ALU.add, [R], [R])
            fw.op(fw.dve, (lambda q=q: nc.vector.reduce_max(out=M2[:, q:q + 1], in_=L2[:, 8 * q:8 * q + 8], axis=AX.X)),
                  [R], [R])
            self.vts(K2[:, 8 * q:8 * q + 8], L2[:, 8 * q:8 * q + 8], M2[:, q:q + 1], ALU.is_equal, [R], [R])
        self.vtt(W2[:, 0:nb], M2[:, 0:nb], M1[:, 0:nb], ALU.subtract, [R], [R])
        self.act(W2[:, 0:nb], W2[:, 0:nb], AF.Exp, [R], [R])
        self.vts(W1[:, 0:nb], W2[:, 0:nb], 1.0, ALU.add, [R], [R])
        fw.op(fw.dve, lambda: nc.vector.reciprocal(W1[:, 0:nb], W1[:, 0:nb]), [R], [R])
        self.vtt(W2[:, 0:nb], W2[:, 0:nb], W1[:, 0:nb], ALU.mult, [R], [R])
        for q in range(nb):
            self.vts(CB[:, 8 * q:8 * q + 8], K1[:, 8 * q:8 * q + 8], W1[:, q:q + 1], ALU.mult, [R], [self.RCB])
            self.vstt(CB[:, 8 * q:8 * q + 8], K2[:, 8 * q:8 * q + 8], W2[:, q:q + 1], CB[:, 8 * q:8 * q + 8],
                      ALU.mult, ALU.add, [R, self.RCB], [self.RCB])

    def proj_T(self, g, l, wsrc, nk, src_fn, src_res, evac):
        slot, rs = self.wst.get(wsrc, nk)
        TS = g["TS"]
        for t in range(g["NT"]):
            b = self.bank()
            self.mm_acc(self.ps[b][:, 0:TS], [(slot[:, k, :], src_fn(k, t)) for k in range(nk)],
                        [rs] + list(src_res), [self.RP[b]])
            evac(t, b)
        return slot, rs

    def pool_phase(self, g, l):
        fw, nc = self.fw, self.nc
        FA, BA, Hh, PO = self.FA, self.BA, self.Hh, self.PO
        T, TS, NT = g["T"], g["TS"], g["NT"]
        segs = g["segs"]
        nseg = len(segs)
        SL = segs[0][1]
        UW = 15 + SL
        U = FA[:, 0:nseg * UW]
        RU = Res("U")
        SA = FA[:, 2304:2304 + 15 + TS]
        SB_ = FA[:, 2880:2880 + 15 + TS]
        RSA, RSB = Res("SA"), Res("SB")
        UT = FA[:, 3456:3456 + 512]
        RUT = Res("UT")
        Dd = [BA[:, 0:512], BA[:, 512:1024]]
        RD = [Res("D0"), Res("D1")]
        w_l = self.w_in[l]
        for gi, w in enumerate(POOL_W):
            for si, (s0, sl, bc) in enumerate(segs):
                if g["kind"] == "p":
                    self.vmemset(U[:, si * UW: si * UW + 15], 0.0, [RU])
                else:
                    self.dma_sp("hist", U[:, si * UW: si * UW + 15], self.spT[l, si, gi], (), [RU])
            def evac(t, b, gi=gi):
                for (a, bnd, bc) in self.segs_in(g, t):
                    si = a // SL
                    off = si * UW + 15 + (a - si * SL)
                    self.acopy(U[:, off: off + (bnd - a)], self.ps[b][:, a - t * TS: bnd - t * TS],
                               [self.RP[b]], [RU])
            slot, rs = self.proj_T(g, l, wblock(w_l, 0, 8, gi * 128), 8,
                                   lambda k, t: Hh[:, k, t * TS:(t + 1) * TS], self.RH, evac)
            b = self.bank()
            self.mm_acc(self.ps[b][:, 0:128], [(Hh[:, k, T - 128:T], slot[:, k, :]) for k in range(8)],
                        [rs] + self.RH, [self.RP[b]])
            self.vcopy(UT[:, gi * 128:(gi + 1) * 128], self.ps[b][:, 0:128], [self.RP[b]], [RUT])
            self.wst.free()
            pw, rpw = self.wst.get(self.pool_w[l, gi].rearrange("(i p) c -> p i c", p=128), 1)
            for si, (s0, sl, bc) in enumerate(segs):
                nsub = max(1, sl // TS)
                L = min(sl, TS)
                for tt in range(nsub):
                    base = si * UW + tt * TS
                    FV = U[:, base: base + 15 + L]
                    Wd = 15 + L
                    cur, rcur = FV, RU
                    bufs = [(SA, RSA), (SB_, RSB)]
                    sh = 1
                    for m in range(gi + 1):
                        lo = 2 * sh - 1
                        dst, rdst = bufs[m % 2]
                        self.vtt(dst[:, lo:Wd], cur[:, lo:Wd], cur[:, lo - sh:Wd - sh], ALU.add, [rcur], [rdst])
                        cur, rcur = dst, rdst
                        sh *= 2
                    di = (tt + si) % 2
                    dd = Dd[di][:, 0:L]
                    self.vstt(dd, cur[:, 15:Wd], 1.0 / w, FV[:, 15:Wd], ALU.mult, ALU.subtract, [rcur, RU], [RD[di]])
                    if g["kind"] == "p" and tt == 0:
                        tmp = SB_[:, 0:16] if cur is SA else SA[:, 0:16]
                        rtmp = RSB if cur is SA else RSA
                        self.vtt(tmp, cur[:, 15:31], self.INVC[:, gi, :], ALU.mult, [rcur, self.RC], [rtmp])
                        self.vtt(dd[:, 0:16], tmp, FV[:, 15:31], ALU.subtract, [rtmp, RU], [RD[di]])
                    b = self.bank()
                    self.mm_acc(self.ps[b][:, 0:L], [(pw[:, 0, :], dd)], [rpw, RD[di]], [self.RP[b]])
                    c0 = s0 + tt * TS
                    self.act(PO[:, gi, c0:c0 + L], self.ps[b][:, 0:L], AF.Copy, [self.RP[b], self.RC], [self.RPO[gi]],
                             scale=self.t_pscale[:, l, gi:gi + 1])
            self.wst.free()
        if g["kind"] == "p":
            self.dma_sp("o_np", self.npp[l, g["s"]], UT[113:128, :], [RUT], ())
        else:
            for b in range(4):
                self.dma_sp("o_np", self.nps[l, b], UT[32 * b + 17:32 * b + 32, :], [RUT], ())

    def attn_finalize(self, g, l, h, c0, n, accb, sbank):
        FA = self.FA
        F = [FA[:, 2048 + 512 * i: 2048 + 512 * i + n] for i in range(4)]
        R = [Res(f"fin{i}") for i in range(4)]
        o0b, o1b, z0b, z1b = accb
        nc, fw = self.nc, self.fw
        fw.op(fw.dve, lambda: nc.vector.reciprocal(F[0], self.ps[z0b][:, 0:n]), [self.RP[z0b]], [R[0]])
        fw.op(fw.dve, lambda: nc.vector.reciprocal(F[1], self.ps[z1b][:, 0:n]), [self.RP[z1b]], [R[1]])
        self.vtt(F[0], self.ps[o0b][:, 0:n], F[0], ALU.mult, [self.RP[o0b], R[0]], [R[0]])
        self.vtt(F[1], self.ps[o1b][:, 0:n], F[1], ALU.mult, [self.RP[o1b], R[1]], [R[1]])
        self.vstt(F[2], F[1], self.LAMT[:, l, 0:1], F[0], ALU.mult, ALU.add, [R[0], R[1], self.RC], [R[2]])
        self.vtt(F[3], F[2], F[2], ALU.mult, [R[2]], [R[3]])
        self.mm_acc(self.ps[sbank][:, 0:n], [(self.ones_rms[:], F[3])], [R[3], self.RC], [self.RP[sbank]])
        self.act(F[0], self.ps[sbank][:, 0:n], AF.Ln, [self.RP[sbank], self.RC], [R[0]], bias=self.eps[:, 1:2], scale=1.0)
        self.act(F[0], F[0], AF.Exp, [R[0]], [R[0]], scale=-0.5)
        self.vstt(self.AO[:, h, c0:c0 + n], F[2], self.LAMT[:, l, 1:2], F[0], ALU.mult, ALU.mult,
                  [R[2], R[0], self.RC], [self.RAO[h]])

    def attn_prompt(self, g, l):
        fw, nc = self.fw, self.nc
        FA, BA, Hh = self.FA, self.BA, self.Hh
        T, TS, NT, s = g["T"], g["TS"], g["NT"], g["s"]
        NB = T // 128
        QT = BA[:, 0:T]
        KT = BA[:, 2048:2048 + T]
        V = BA[:, 4096:4096 + T]
        RQ, RK, RV = Res("QT"), Res("KT"), Res("V")
        E = [BA[:, 6144 + 512 * i: 6144 + 512 * (i + 1)] for i in range(4)]
        RE = [Res(f"E{i}") for i in range(4)]
        KST = [FA[:, 0:512], FA[:, 0:512]]
        VST = [FA[:, 512:1024], FA[:, 512:1024]]
        _rk, _rv = Res("kst"), Res("vst")
        RKS = [_rk, _rk]
        RVS = [_rv, _rv]
        w_l = self.w_in[l]
        ecnt = 0
        for h in range(NH):
            wi = h % 2
            self.dma_sp(f"wh{wi}", self.WH[wi][:], self.wscr.ap()[h], [self.RSCR], [self.RWH[wi]])
            WHh, RWHh = self.WH[wi], self.RWH[wi]
            self.chk("wh")
            self.proj_T(g, l, wblock(w_l, 0, 8, O_Q + h * 128), 8, lambda k, t: Hh[:, k, t * TS:(t + 1) * TS], self.RH,
                        lambda t, b: self.acopy(QT[:, t * TS:(t + 1) * TS], self.ps[b][:, 0:TS], [self.RP[b]], [RQ]))
            kslot, krs = self.proj_T(g, l, wblock(w_l, 0, 8, O_K + h * 128), 8,
                                     lambda k, t: Hh[:, k, t * TS:(t + 1) * TS], self.RH,
                                     lambda t, b: self.acopy(KT[:, t * TS:(t + 1) * TS], self.ps[b][:, 0:TS],
                                                             [self.RP[b]], [RK]))
            self.chk("qk")
            for q4 in range(NB // 4):
                b = self.bank()
                for qq in range(4):
                    blk = q4 * 4 + qq
                    self.mm_acc(self.ps[b][:, 128 * qq:128 * qq + 128],
                                [(Hh[:, k, blk * 128:(blk + 1) * 128], kslot[:, k, :]) for k in range(8)],
                                [krs] + self.RH, [self.RP[b]])
                si = q4 % 2
                self.vcopy(KST[si], self.ps[b][:, 0:512], [self.RP[b]], [RKS[si]])
                self.chk("ktok")
                self.dma_sp(f"o_k{si}", self.nkp[l, s, q4 * 512:(q4 + 1) * 512, h, :].rearrange("(b p) d -> p b d", p=128),
                            KST[si].rearrange("p (b d) -> p b d", b=4), [RKS[si]], ())
            self.chk("kdma")
            vslot, vrs = self.wst.get(wblock(w_l, 0, 8, O_V + h * 128), 8)
            for q4 in range(NB // 4):
                b = self.bank()
                for qq in range(4):
                    blk = q4 * 4 + qq
                    self.mm_acc(self.ps[b][:, 128 * qq:128 * qq + 128],
                                [(Hh[:, k, blk * 128:(blk + 1) * 128], vslot[:, k, :]) for k in range(8)],
                                [vrs] + self.RH, [self.RP[b]])
                si = q4 % 2
                self.acopy(V[:, q4 * 512:(q4 + 1) * 512], self.ps[b][:, 0:512], [self.RP[b]], [RV])
                self.vcopy(VST[si], self.ps[b][:, 0:512], [self.RP[b]], [RVS[si]])
                self.dma_sp(f"o_v{si}", self.nvp[l, s, q4 * 512:(q4 + 1) * 512, h, :].rearrange("(b p) d -> p b d", p=128),
                            VST[si].rearrange("p (b d) -> p b d", b=4), [RVS[si]], ())
            self.wst.free()
            self.chk("v")
            for t in range(NT):
                nbq = TS // 128
                jmax = nbq * t + nbq - 1
                for j in range(jmax + 1):
                    dl = j - nbq * t
                    q0 = max(0, 128 * dl)
                    sb0, sb1 = (4, 5) if (ecnt % 2 == 0) else (6, 7)
                    e0, e1 = (0, 1) if (ecnt % 2 == 0) else (2, 3)
                    ecnt += 1
                    qs = t * TS + q0
                    self.fw.mm_group(
                        [self.mm(self.ps[sb0][:, q0:TS], KT[0:64, j * 128:(j + 1) * 128], QT[0:64, qs:(t + 1) * TS], True, True),
                         self.mm(self.ps[sb1][:, q0:TS], KT[64:128, j * 128:(j + 1) * 128], QT[64:128, qs:(t + 1) * TS], True, True)],
                        [RK, RQ], [self.RP[sb0], self.RP[sb1]])
                    self.chk("S")
                    far = dl <= -2
                    for (sbk, ei) in ((sb0, e0), (sb1, e1)):
                        if far:
                            self.act(E[ei][:, q0:TS], self.ps[sbk][:, q0:TS], AF.Exp, [self.RP[sbk], self.RC], [RE[ei]],
                                     bias=self.BF15[:, h:h + 1], scale=0.125)
                        else:
                            xi = ei % 2
                            self.act(self.EX[xi][:, q0:TS], self.ps[sbk][:, q0:TS], AF.Exp, [self.RP[sbk]], [self.REX[xi]], scale=0.125)
                            if True:
                                self.vtt(E[ei][:, q0:TS], self.EX[xi][:, q0:TS], WHh[:, q0 - 128 * dl: TS - 128 * dl], ALU.mult,
                                         [self.REX[xi], RWHh], [RE[ei]])
                    self.chk("E")
                    st, sp_ = (j == 0), (j == jmax)
                    self.fw.mm_group(
                        [self.mm(self.ps[0][:, q0:TS], V[:, j * 128:(j + 1) * 128], E[e0][:, q0:TS], st, sp_),
                         self.mm(self.ps[2][:, q0:TS], self.ones_b[:], E[e0][:, q0:TS], st, sp_),
                         self.mm(self.ps[1][:, q0:TS], V[:, j * 128:(j + 1) * 128], E[e1][:, q0:TS], st, sp_),
                         self.mm(self.ps[3][:, q0:TS], self.ones_b[:], E[e1][:, q0:TS], st, sp_)],
                        [RV, RE[e0], RE[e1], self.RC], [self.RP[0], self.RP[1], self.RP[2], self.RP[3]])
                self.chk("AV")
                sbank = 4 if (ecnt % 2 == 0) else 6
                self.attn_finalize(g, l, h, t * TS, TS, (0, 1, 2, 3), sbank)
                self.chk("fin")

    def attn_sample(self, g, l):
        fw, nc = self.fw, self.nc
        FA, BA, Hh = self.FA, self.BA, self.Hh
        QT = BA[:, 0:128]
        KT = BA[:, 128:256]
        RQ, RK = Res("QT"), Res("KT")
        VN = [BA[0:32, 256 + 128 * b: 384 + 128 * b] for b in range(4)]
        RVN = Res("VN")
        E = [BA[:, 1024 + 512 * i: 1024 + 512 * (i + 1)] for i in range(4)]
        RE = [Res(f"E{i}") for i in range(4)]
        KST = FA[0:32, 0:512]
        VST = FA[0:32, 512:1024]
        RKS, RVS = Res("kst"), Res("vst")
        w_l = self.w_in[l]
        ecnt = 0
        for h in range(NH):
            wi = h % 2
            self.dma_sp(f"wh{wi}", self.WH[wi][:], self.wscr.ap()[h], [self.RSCR], [self.RWH[wi]])
            WHh, RWHh = self.WH[wi], self.RWH[wi]
            self.proj_T(g, l, wblock(w_l, 0, 8, O_Q + h * 128), 8, lambda k, t: Hh[:, k, 0:128], self.RH,
                        lambda t, b: self.acopy(QT, self.ps[b][:, 0:128], [self.RP[b]], [RQ]))
            kslot, krs = self.proj_T(g, l, wblock(w_l, 0, 8, O_K + h * 128), 8, lambda k, t: Hh[:, k, 0:128], self.RH,
                                     lambda t, b: self.acopy(KT, self.ps[b][:, 0:128], [self.RP[b]], [RK]))
            b = self.bank()
            for bb in range(4):
                self.mm_acc(self.ps[b][0:32, 128 * bb:128 * bb + 128],
                            [(Hh[:, k, 32 * bb:32 * bb + 32], kslot[:, k, :]) for k in range(8)],
                            [krs] + self.RH, [self.RP[b]])
            self.vcopy(KST, self.ps[b][0:32, 0:512], [self.RP[b]], [RKS])
            for bb in range(4):
                self.dma_sp("o_k0", self.nks[l, bb, :, h, :], KST[:, 128 * bb:128 * bb + 128], [RKS], ())
            vslot, vrs = self.wst.get(wblock(w_l, 0, 8, O_V + h * 128), 8)
            b = self.bank()
            for bb in range(4):
                self.mm_acc(self.ps[b][0:32, 128 * bb:128 * bb + 128],
                            [(Hh[:, k, 32 * bb:32 * bb + 32], vslot[:, k, :]) for k in range(8)],
                            [vrs] + self.RH, [self.RP[b]])
            self.acopy(BA[0:32, 256:768], self.ps[b][0:32, 0:512], [self.RP[b]], [RVN])
            self.vcopy(VST, self.ps[b][0:32, 0:512], [self.RP[b]], [RVS])
            for bb in range(4):
                self.dma_sp("o_v0", self.nvs[l, bb, :, h, :], VST[:, 128 * bb:128 * bb + 128], [RVS], ())
            self.wst.free()
            for bb in range(4):
                kc_slot, kcr = self.wst.get(self.ckT[l, bb, h].rearrange("p (a b) -> p a b", b=128), 8)
                vc_slot, vcr = self.wst.get(self.cv[l, bb, :, h, :].rearrange("(j p) d -> p j d", p=128), 8)
                sb0, sb1 = (4, 5) if (ecnt % 2 == 0) else (6, 7)
                e0, e1 = (0, 1) if (ecnt % 2 == 0) else (2, 3)
                ecnt += 1
                qcol = slice(32 * bb, 32 * bb + 32)
                fns = []
                for j in range(8):
                    fns.append(self.mm(self.ps[sb0][:, 32 * j:32 * j + 32], kc_slot[0:64, j, :], QT[0:64, qcol], True, True))
                    fns.append(self.mm(self.ps[sb1][:, 32 * j:32 * j + 32], kc_slot[64:128, j, :], QT[64:128, qcol], True, True))
                fns.append(self.mm(self.ps[sb0][0:32, 256:288], KT[0:64, qcol], QT[0:64, qcol], True, True))
                fns.append(self.mm(self.ps[sb1][0:32, 256:288], KT[64:128, qcol], QT[64:128, qcol], True, True))
                self.fw.mm_group(fns, [kcr, RQ, RK], [self.RP[sb0], self.RP[sb1]])
                for (sbk, ei) in ((sb0, e0), (sb1, e1)):
                    self.act(E[ei][:, 0:224], self.ps[sbk][:, 0:224], AF.Exp, [self.RP[sbk], self.RC], [RE[ei]],
                             bias=self.BF15[:, h:h + 1], scale=0.125)
                    xi = ei % 2
                    EXt = self.EX[xi]
                    self.act(EXt[:, 224:256], self.ps[sbk][:, 224:256], AF.Exp, [self.RP[sbk]], [self.REX[xi]], scale=0.125)
                    self.act(EXt[0:32, 256:288], self.ps[sbk][0:32, 256:288], AF.Exp, [self.RP[sbk]], [self.REX[xi]], scale=0.125)
                    self.vtt(E[ei][:, 224:256], EXt[:, 224:256], WHh[:, 128:160], ALU.mult, [self.REX[xi], RWHh], [RE[ei]])
                    self.vtt(E[ei][0:32, 256:288], EXt[0:32, 256:288], WHh[0:32, 0:32], ALU.mult, [self.REX[xi], RWHh], [RE[ei]])
                fns = []
                for ci, (ob, zb, ei) in enumerate(((0, 2, e0), (1, 3, e1))):
                    for j in range(8):
                        fns.append(self.mm(self.ps[ob][:, qcol], vc_slot[:, j, :], E[ei][:, 32 * j:32 * j + 32], j == 0, False))
                    fns.append(self.mm(self.ps[ob][:, qcol], VN[bb], E[ei][0:32, 256:288], False, True))
                    for j in range(8):
                        fns.append(self.mm(self.ps[zb][:, qcol], self.ones_b[:], E[ei][:, 32 * j:32 * j + 32], j == 0, False))
                    fns.append(self.mm(self.ps[zb][:, qcol], self.ones_b[0:32, :], E[ei][0:32, 256:288], False, True))
                self.fw.mm_group(fns, [vcr, RVN, RE[e0], RE[e1], self.RC], [self.RP[0], self.RP[1], self.RP[2], self.RP[3]])
                self.wst.free()
            sbank = 4 if (ecnt % 2 == 0) else 6
            self.attn_finalize(g, l, h, 0, 128, (0, 1, 2, 3), sbank)

    def merge_phase(self, g, l):
        FA, BA, Hh, AO, PO, X = self.FA, self.BA, self.Hh, self.AO, self.PO, self.X
        T, TS, NT = g["T"], g["TS"], g["NT"]
        w_l = self.w_in[l]
        halves = [(0, T)] if T <= 1024 else [(0, T // 2), (T // 2, T)]
        TM = [FA[:, 512 * i: 512 * (i + 1)] for i in range(4)]
        RTM = [Res(f"tm{i}") for i in range(4)]
        RMg = [Res(f"M{k}") for k in range(8)]
        for (h0, h1) in halves:
            HL = h1 - h0
            tiles = [(c, min(c + TS, h1)) for c in range(h0, h1, TS)]

            def Mv(kc, a, b):
                return BA[:, kc * HL + (a - h0): kc * HL + (b - h0)]
            cnt = 0
            for j in range(8):
                gp, rgp = self.wst.get(wblock(w_l, 0, 8, O_GP + j * 128), 8)
                ga, rga = self.wst.get(wblock(w_l, 0, 8, O_GA + j * 128), 8)
                pu, rpu = self.wst.get(wblock(self.w_pool_up[l], 0, 4, j * 128), 4)
                au, rau = self.wst.get(wblock(self.w_attn_up[l], 0, 8, j * 128), 8)
                for (a, b) in tiles:
                    n = b - a
                    b1, b2, b3, b4 = self.bank(), self.bank(), self.bank(), self.bank()
                    self.mm_acc(self.ps[b1][:, 0:n], [(gp[:, k, :], Hh[:, k, a:b]) for k in range(8)], [rgp] + self.RH, [self.RP[b1]])
                    self.mm_acc(self.ps[b2][:, 0:n], [(ga[:, k, :], Hh[:, k, a:b]) for k in range(8)], [rga] + self.RH, [self.RP[b2]])
                    self.mm_acc(self.ps[b3][:, 0:n], [(pu[:, k, :], PO[:, k, a:b]) for k in range(4)], [rpu] + self.RPO, [self.RP[b3]])
                    self.mm_acc(self.ps[b4][:, 0:n], [(au[:, k, :], AO[:, k, a:b]) for k in range(8)], [rau] + self.RAO, [self.RP[b4]])
                    i0 = (cnt % 2) * 2
                    cnt += 1
                    t1, t2 = TM[i0][:, 0:n], TM[i0 + 1][:, 0:n]
                    r1, r2 = RTM[i0], RTM[i0 + 1]
                    self.act(t1, self.ps[b1][:, 0:n], AF.Sigmoid, [self.RP[b1], self.RC], [r1], bias=self.t_bgate[:, l, j:j + 1], scale=1.0)
                    self.act(t2, self.ps[b2][:, 0:n], AF.Sigmoid, [self.RP[b2], self.RC], [r2], bias=self.t_bgate[:, l, 8 + j:9 + j], scale=1.0)
                    self.vtt(t1, self.ps[b3][:, 0:n], t1, ALU.mult, [self.RP[b3], r1], [r1])
                    self.vtt(t2, self.ps[b4][:, 0:n], t2, ALU.mult, [self.RP[b4], r2], [r2])
                    self.vtt(Mv(j, a, b), t1, t2, ALU.add, [r1, r2], [RMg[j]])
                self.wst.free()
            for j in range(8):
                wo, rwo = self.wst.get(wblock(self.w_o[l], 0, 8, j * 128), 8)
                for (a, b) in tiles:
                    n = b - a
                    bk = self.bank()
                    self.mm_acc(self.ps[bk][:, 0:n], [(wo[:, k, :], Mv(k, a, b)) for k in range(8)], [rwo] + RMg, [self.RP[bk]])
                    self.x_update(g, l, 0, j, a, b, bk, first=True)
                self.wst.free()
            self.fw.barrier()

    def x_update(self, g, l, which, j, a, b, bk, first):
        gate0 = 16 if which == 0 else 40
        for (s0, sl, bc) in g["segs"]:
            lo, hi = max(a, s0), min(b, s0 + sl)
            if lo >= hi:
                continue
            gate = self.MOD[:, l, gate0 + j, bc:bc + 1]
            pv = self.ps[bk][:, lo - a:hi - a]
            xv = self.X[:, j, lo:hi]
            if first:
                tmpi = self.xu_cnt % 2
                self.xu_cnt += 1
                tmp = self.FA[:, 3072 + 512 * tmpi: 3072 + 512 * tmpi + (hi - lo)]
                rt = self.RXU[tmpi]
                self.act(tmp, pv, AF.Copy, [self.RP[bk], self.RC], [rt], scale=gate)
                self.vstt(xv, xv, ALPHA, tmp, ALU.mult, ALU.add, [self.RX[j], rt], [self.RX[j]])
            else:
                self.vstt(xv, pv, gate, xv, ALU.mult, ALU.add, [self.RP[bk], self.RX[j], self.RC], [self.RX[j]])

    def ffn_phase(self, g, l):
        FA, BA, Hh, AO, X = self.FA, self.BA, self.Hh, self.AO, self.X
        T, TS, NT = g["T"], g["TS"], g["NT"]
        if l == 0:
            experts = [(self.wfg[0], self.wfu[0], self.wfd[0], DFF // 128, None)]
        else:
            experts = [(self.weg[0, e], self.weu[0, e], self.wed[0, e], DFE // 128, e) for e in range(NE)]
        SG = [FA[:, 0:512], FA[:, 512:1024]]
        RSG = [Res("sg0"), Res("sg1")]
        BC = FA[:, 1024:1024 + T]
        RBC = Res("BC")
        DG = [FA[:, 4096:4224], FA[:, 4224:4352]]
        RDG = [Res("dg0"), Res("dg1")]
        RA = [Res(f"A{k}") for k in range(8)]
        first = True
        cnt = 0
        for (wg, wu, wd, nff, e) in experts:
            if e is not None:
                for q4 in range(max(1, T // 512)):
                    bk = self.bank()
                    nq = min(4, T // 128)
                    for qq in range(nq):
                        blk = q4 * 4 + qq
                        di = blk % 2
                        self.vts(DG[di], self.ident[:], self.CBall[:, blk * 8 + e: blk * 8 + e + 1], ALU.mult,
                                 [self.RC, self.RCB], [RDG[di]])
                        self.mm_acc(self.ps[bk][:, 128 * qq:128 * qq + 128], [(self.ones_f[:], DG[di])],
                                    [self.RC, RDG[di]], [self.RP[bk]])
                    self.vcopy(BC[:, q4 * 512: q4 * 512 + 128 * nq], self.ps[bk][:, 0:128 * nq], [self.RP[bk]], [RBC])
            ngrp = (nff + 7) // 8
            base, rem = nff // ngrp, nff % ngrp
            i0 = 0
            for gi in range(ngrp):
                gsz = base + (1 if gi < rem else 0)
                for il in range(gsz):
                    i = i0 + il
                    sg_, rsg = self.wst.get(wblock(wg, 0, 8, i * 128), 8)
                    su_, rsu = self.wst.get(wblock(wu, 0, 8, i * 128), 8)
                    for t in range(NT):
                        c0, c1 = t * TS, (t + 1) * TS
                        b1, b2 = self.bank(), self.bank()
                        self.mm_acc(self.ps[b1][:, 0:TS], [(sg_[:, k, :], Hh[:, k, c0:c1]) for k in range(8)], [rsg] + self.RH, [self.RP[b1]])
                        self.mm_acc(self.ps[b2][:, 0:TS], [(su_[:, k, :], Hh[:, k, c0:c1]) for k in range(8)], [rsu] + self.RH, [self.RP[b2]])
                        si = cnt % 2
                        cnt += 1
                        s_, rs_ = SG[si][:, 0:TS], RSG[si]
                        self.act(s_, self.ps[b1][:, 0:TS], AF.Silu, [self.RP[b1]], [rs_])
                        if e is None:
                            self.vtt(AO[:, il, c0:c1], s_, self.ps[b2][:, 0:TS], ALU.mult, [rs_, self.RP[b2]], [RA[il]])
                        else:
                            self.vtt(s_, s_, self.ps[b2][:, 0:TS], ALU.mult, [rs_, self.RP[b2]], [rs_])
                            self.vtt(AO[:, il, c0:c1], s_, BC[:, c0:c1], ALU.mult, [rs_, RBC], [RA[il]])
                    self.wst.free()
                for j in range(8):
                    sd_, rsd = self.wst.get(wblock(wd, i0 * 128, gsz, j * 128), gsz)
                    for t in range(NT):
                        c0, c1 = t * TS, (t + 1) * TS
                        bk = self.bank()
                        self.mm_acc(self.ps[bk][:, 0:TS], [(sd_[:, il, :], AO[:, il, c0:c1]) for il in range(gsz)],
                                    [rsd] + RA[0:gsz], [self.RP[bk]])
                        self.x_update(g, l, 1, j, c0, c1, bk, first=first)
                    self.wst.free()
                first = False
                i0 += gsz

    def write_y(self, g):
        FA, X = self.FA, self.X
        nc = self.nc
        T = g["T"]
        YS = [FA[:, 0:1024], FA[:, 1024:2048]]
        RY = [Res("ys0"), Res("ys1")]
        for blk in range(T // 128):
            si = blk % 2
            for half in range(2):
                bk = self.bank()
                self.fw.mm_group([self.tr(self.ps[bk][:, 128 * (kc % 4):128 * (kc % 4) + 128],
                                          X[:, kc, blk * 128:(blk + 1) * 128])
                                  for kc in range(4 * half, 4 * half + 4)],
                                 self.RX[4 * half:4 * half + 4] + [self.RC], [self.RP[bk]])
                if half == 0:
                    self.vcopy(YS[si][:, 0:512], self.ps[bk][:, 0:512], [self.RP[bk]], [RY[si]])
                else:
                    self.acopy(YS[si][:, 512:1024], self.ps[bk][:, 0:512], [self.RP[bk]], [RY[si]])
            if g["kind"] == "p":
                dst = self.yp[g["s"], blk * 128:(blk + 1) * 128, :]
            else:
                dst = self.ys
            self.dma_sp(f"o_y{si}", dst, YS[si], [RY[si]], ())

    def run_group(self, g):
        fw = self.fw
        X = self.X
        T = g["T"]
        self.CBall = self.FA[:, 4480:4608]
        self.RCB = Res("CB")
        self.xu_cnt = 0
        self.RXU = [Res("xu0"), Res("xu1")]
        src = self.xp[g["s"]] if g["kind"] == "p" else self.xs
        if g["kind"] == "p":
            for kc in range(8):
                self.dma_sp(f"ldx{kc}", X[:, kc, :], src[kc], (), [self.RX[kc]])
        else:
            for kc in range(8):
                self.dma_sp(f"ldx{kc}", X[:, kc, 0:128], src[kc], (), [self.RX[kc]])
        self.modulate0(g)
        self.tick("mod0")
        for l in range(2):
            fw.barrier()
            self.pool_phase(g, l)
            self.tick("pool")
            fw.barrier()
            if g["kind"] == "p":
                self.attn_prompt(g, l)
            else:
                self.attn_sample(g, l)
            self.tick("attn")
            fw.barrier()
            self.merge_phase(g, l)
            self.tick("merge")
            fw.barrier()
            self.layernorm(g, l, 0)
            self.tick("ln0")
            fw.barrier()
            self.ffn_phase(g, l)
            self.tick("ffn")
            fw.barrier()
            self.layernorm(g, l, 1)
            self.tick("ln1")
            fw.barrier()
        self.write_y(g)
        self.tick("y")
        fw.barrier()

    def body(self):
        import os
        self.bank_rr = 0
        self._it = 0
        self._chk = 0
        self.phase_no = 0
        self.stop_at = int(os.environ.get("K_STOP", "100000"))
        try:
            self.setup()
            self.tick("setup")
            order = os.environ.get("K_ORDER", "ps")
            for ch in order:
                if ch == "p":
                    for s in range(self.n_seq):
                        self.run_group(self.make_group("p", s))
                elif self.with_sample:
                    self.run_group(self.make_group("s", 0))
        except StopIteration:
            pass

    def chk(self, name=""):
        self._chk = getattr(self, "_chk", 0) + 1
        lim = int(os.environ.get("K_CHK", "0"))
        if lim and self._chk >= lim:
            if not self.fw.dry:
                print("CHK stop at", self._chk, name)
            raise StopIteration

    def tick(self, name):
        self.phase_no += 1
        if not self.fw.dry and os.environ.get("K_VERBOSE"):
            print("phase", self.phase_no, name, "ins", self.fw.n_ins, "waits", self.fw.n_wait)
        if self.phase_no >= self.stop_at:
            raise StopIteration

    def build(self):
        fw = self.fw
        fw.dry = True
        self.body()
        fw.dry = False
        self.wst.reset_for_real()
        self.body()
        fw.barrier(engines=("sp",))
        fw.flush()
        return self.nc


def _tab(v, n):
    return np.ascontiguousarray(np.asarray(v, np.float32).reshape(n, 128).T)


def prep_shared(inp):
    f = lambda a: np.ascontiguousarray(np.asarray(a, np.float32))
    sh = {}
    sh["relb"] = f(inp["rel_bias"])
    sh["ohm"] = _bucket_onehot()
    sh["w_ada"] = f(inp["w_ada"])
    sh["b_ada_t"] = np.ascontiguousarray(np.stack([_tab(inp["b_ada"][l], 48) for l in range(2)], axis=1))
    sh["w_in"] = f(inp["w_in"])
    sh["b_gate_t"] = np.ascontiguousarray(np.stack([_tab(inp["b_gate"][l], 16) for l in range(2)], axis=1))
    sh["pool_w"] = f(inp["pool_w"])
    sh["pscale_t"] = np.ascontiguousarray(np.stack([_tab(inp["pool_scale"][l], 4) for l in range(2)], axis=1))
    lq = np.asarray(inp["lam_qk"], np.float32).reshape(1, 2, 256)
    sh["lamqk_b"] = np.ascontiguousarray(np.broadcast_to(lq, (128, 2, 256)))
    sh["subln_t"] = np.ascontiguousarray(np.asarray(inp["subln_g"], np.float32).T)
    sh["w_pool_up"] = f(inp["w_pool_up"])
    sh["w_attn_up"] = f(inp["w_attn_up"])
    sh["w_o"] = f(inp["w_o"])
    lg = np.asarray(inp["ln_g"], np.float32).reshape(2, 2, 8, 128)
    lb = np.asarray(inp["ln_b"], np.float32).reshape(2, 2, 8, 128)
    sh["lng_t"] = np.ascontiguousarray(lg.transpose(3, 0, 1, 2))
    sh["lnb_t"] = np.ascontiguousarray(lb.transpose(3, 0, 1, 2))
    sh["w_ffn_gate"] = f(inp["w_ffn_gate"])
    sh["w_ffn_up"] = f(inp["w_ffn_up"])
    sh["w_ffn_down"] = f(inp["w_ffn_down"])
    wr = np.asarray(inp["w_router"], np.float32)[0].reshape(8, 128, 8)
    sh["w_router_t"] = np.ascontiguousarray(wr.transpose(1, 0, 2))
    sh["w_exp_gate"] = f(inp["w_exp_gate"])
    sh["w_exp_up"] = f(inp["w_exp_up"])
    sh["w_exp_down"] = f(inp["w_exp_down"])
    return sh


def prep_core(inp, pb, sb_, T):
    m = {}
    xp = np.asarray(inp["x_prompt"], np.float32)[pb][:, :T]
    m["xp"] = np.ascontiguousarray(xp.transpose(0, 2, 1)).reshape(len(pb), 8, 128, T)
    xs = np.asarray(inp["x_sample"], np.float32)[sb_].reshape(128, 1024)
    m["xs"] = np.ascontiguousarray(xs.T).reshape(8, 128, 128)
    ck = np.asarray(inp["cache_k"], np.float32)[:, sb_]
    m["ckT"] = np.ascontiguousarray(ck.transpose(0, 1, 3, 4, 2))
    m["cv"] = np.ascontiguousarray(np.asarray(inp["cache_v"], np.float32)[:, sb_])
    sp = np.asarray(inp["state_pool"], np.float32)[:, sb_]
    m["spT"] = np.ascontiguousarray(sp.transpose(0, 1, 3, 2)).reshape(2, 4, 4, 128, 15)
    cp = np.asarray(inp["c_prompt"], np.float32)[pb]
    if len(pb) < 4:
        cp = np.concatenate([cp, np.zeros((4 - len(pb), 1024), np.float32)], 0)
    cs = np.asarray(inp["c_sample"], np.float32)[sb_]
    call = np.concatenate([cp, cs], 0)
    m["cT"] = np.ascontiguousarray(call.T.reshape(8, 128, 8).transpose(1, 0, 2)).reshape(128, 64)
    return m


_NC_CACHE = {}


def get_program(n_seq, T, with_sample):
    key = (n_seq, T, with_sample)
    if key not in _NC_CACHE:
        _NC_CACHE[key] = Builder(n_seq, T, with_sample).build()
    return _NC_CACHE[key]


def kernel(**inputs):
    nc = get_program(4, SEQ, True)
    shared = prep_shared(inputs)
    in_maps = []
    for c in range(NCORES):
        idx = list(range(4 * c, 4 * c + 4))
        m = dict(shared)
        m.update(prep_core(inputs, idx, idx, SEQ))
        in_maps.append(m)
    res = run_bass_kernel_spmd(nc, in_maps, core_ids=list(range(NCORES)))
    r = res.results
    y_prompt = np.concatenate([x["yp"] for x in r], 0)
    y_sample = np.concatenate([x["ys"].reshape(4, 32, D) for x in r], 0)
    nkp = np.concatenate([x["nkp"] for x in r], 1)
    nvp = np.concatenate([x["nvp"] for x in r], 1)
    npp = np.concatenate([x["npp"] for x in r], 1)
    nks = np.concatenate([x["nks"] for x in r], 1)
    nvs = np.concatenate([x["nvs"] for x in r], 1)
    nps = np.concatenate([x["nps"] for x in r], 1)
    return tuple(np.ascontiguousarray(a.astype(np.float32, copy=False))
                 for a in (y_prompt, y_sample, nkp, nvp, npp, nks, nvs, nps))
```

```python
import math
import os
import numpy as np
import concourse.bass as bass
import concourse.mybir as mybir
from concourse.bass_utils import run_bass_kernel_spmd

F32 = mybir.dt.float32
BF16 = mybir.dt.bfloat16
AF = mybir.ActivationFunctionType
ALU = mybir.AluOpType
AX = mybir.AxisListType

D = 1024
NKC = 8
NH = 8
DEPTH = 2
SEQ = 2048
DEC_SEQ = 32
PAST = 1024
IN_W = 5632
O_Q, O_K, O_V, O_GP, O_GA = 512, 1536, 2560, 3584, 4608
DFF = 2816
DFE = 3584
NE = 8
ALPHA = (2.0 * DEPTH) ** 0.25
LN_EPS = 1e-5
RMS_EPS = 1e-5
POOL_W = (2, 4, 8, 16)
LAM_INIT = [0.8 - 0.6 * math.exp(-0.3 * l) for l in range(DEPTH)]
N_BUCKETS = 32
MAX_DISTANCE = 128
NCORES = 8

EPOCH = 16384
SAME_ENGINE_SYNC = True


class Res:
    __slots__ = ("name", "last_w", "reads", "excl")

    def __init__(self, name, excl=False):
        self.name = name
        self.last_w = None
        self.reads = []
        self.excl = excl


class Eng:
    def __init__(self, fw, name, is_pe=False):
        self.fw = fw
        self.name = name
        self.is_pe = is_pe
        self.count = 0
        self.sems = []
        self.known = {}
        self.dknown = {}
        self.prog = []

    def sem_for(self, n):
        e = (n - 1) // EPOCH
        while len(self.sems) <= e:
            self.sems.append(self.fw.nc.alloc_semaphore(f"s_{self.name}_{len(self.sems)}"))
        return self.sems[e], (n - 1) % EPOCH + 1


class FW:
    def __init__(self, nc):
        self.nc = nc
        self.dry = False
        self.pe = Eng(self, "pe", True)
        self.act = Eng(self, "act")
        self.dve = Eng(self, "dve")
        self.pool = Eng(self, "pool")
        self.sp = Eng(self, "sp")
        self.engs = {e.name: e for e in (self.pe, self.act, self.dve, self.pool, self.sp)}
        self.dma_sems = {}
        self.n_wait = 0
        self.n_ins = 0

    def _wait(self, eng, ev):
        if ev is None:
            return
        if ev[0] == "e":
            _, ename, n = ev
            if ename == eng.name and (eng.is_pe or not SAME_ENGINE_SYNC):
                return
            if eng.known.get(ename, 0) >= n:
                return
            sem, val = self.engs[ename].sem_for(n)
            self._emit_wait(eng, sem, val)
            eng.known[ename] = n
        else:
            _, key, val = ev
            if eng.dknown.get(key, 0) >= val:
                return
            self._emit_wait(eng, self.dma_sems[key][0], val)
            eng.dknown[key] = val

    def _emit_wait(self, eng, sem, val):
        i = len(eng.prog) - 1
        while i >= 0 and eng.prog[i][0] == "w":
            if eng.prog[i][1] is sem or eng.prog[i][1] == sem:
                if eng.prog[i][2] < val:
                    eng.prog[i] = ("w", sem, val)
                return
            i -= 1
        eng.prog.append(("w", sem, val))
        self.n_wait += 1

    def _deps(self, eng, reads, writes):
        need = {}
        def add(ev):
            if ev is None:
                return
            k = (ev[0], ev[1])
            if k not in need or need[k][2] < ev[2]:
                need[k] = ev
        for r in reads:
            add(r.last_w)
        for w in writes:
            add(w.last_w)
            for ev in w.reads:
                add(ev)
        for ev in need.values():
            self._wait(eng, ev)

    def _record(self, ev, reads, writes):
        for r in reads:
            r.reads.append(ev)
            if len(r.reads) > 40:
                last = {}
                for e in r.reads:
                    last[(e[0], e[1])] = e
                r.reads = list(last.values())
        for w in writes:
            w.last_w = ev
            w.reads = []

    def op(self, eng, fn, reads=(), writes=()):
        if self.dry:
            return
        ex = [r for r in reads if r.excl]
        if ex:
            writes = list(writes) + ex
            reads = [r for r in reads if not r.excl]
        self._deps(eng, reads, writes)
        eng.count += 1
        sem, _ = eng.sem_for(eng.count)
        eng.prog.append(("i", fn, sem, 1))
        self.n_ins += 1
        self._record(("e", eng.name, eng.count), reads, writes)

    def mm_group(self, fns, reads=(), writes=()):
        if self.dry:
            return
        eng = self.pe
        self._deps(eng, reads, writes)
        for fn in fns[:-1]:
            eng.prog.append(("i", fn, None, 0))
        eng.count += 1
        sem, _ = eng.sem_for(eng.count)
        eng.prog.append(("i", fns[-1], sem, 1))
        self.n_ins += len(fns)
        self._record(("e", eng.name, eng.count), reads, writes)

    def dma(self, q, key, out, in_, reads=(), writes=()):
        if self.dry:
            return
        self._deps(q, reads, writes)
        if key not in self.dma_sems:
            self.dma_sems[key] = [self.nc.alloc_semaphore(f"d_{key}"), 0]
        ent = self.dma_sems[key]
        ent[1] += 16
        assert ent[1] < 60000, key
        qn = q.name
        q.prog.append(("d", out, in_, ent[0], 16))
        self.n_ins += 1
        self._record(("d", key, ent[1]), reads, writes)

    def barrier(self, engines=("pe", "act", "dve", "sp")):
        if self.dry:
            return
        for en in engines:
            eng = self.engs[en]
            for other in self.engs.values():
                if other.name == en or other.count == 0:
                    continue
                self._wait(eng, ("e", other.name, other.count))
            for key, ent in self.dma_sems.items():
                if ent[1] > 0:
                    self._wait(eng, ("d", key, ent[1]))

    def dump(self, path):
        with open(path, "w") as f:
            for en, eng in self.engs.items():
                f.write(f"==== {en}\n")
                for i, item in enumerate(eng.prog):
                    if item[0] == "w":
                        f.write(f"{i} WAIT {item[1]} >= {item[2]}\n")
                    elif item[0] == "i":
                        f.write(f"{i} INS inc={item[2]} {item[3]} {getattr(item[1], '_dbg', '')}\n")
                    else:
                        f.write(f"{i} DMA inc={item[3]}\n")

    def flush(self):
        nc = self.nc
        import os
        if os.environ.get("K_DUMP"):
            self.dump(os.environ["K_DUMP"])
        hmap = {"sp": "sync", "pool": "gpsimd", "act": "scalar", "dve": "vector", "pe": "tensor"}
        with nc.Block() as block:
            def run(eng):
                def body(e):
                    for item in eng.prog:
                        k = item[0]
                        if k == "w":
                            e.wait_ge(item[1], item[2])
                        elif k == "i":
                            ins = item[1]()
                            if item[2] is not None:
                                ins.then_inc(item[2], item[3])
                        else:
                            e.dma_start(out=item[1], in_=item[2]).then_inc(item[3], item[4])
                return body
            for en, bn in hmap.items():
                getattr(block, bn)(run(self.engs[en]))


class WStream:
    NSLOT = 8
    LOOK = 4

    def __init__(self, fw, nc):
        self.fw = fw
        self.slots = [nc.alloc_sbuf_tensor(f"wslot{i}", [128, 8, 128], BF16) for i in range(self.NSLOT)]
        self.res = [Res(f"wslot{i}") for i in range(self.NSLOT)]
        self.reqs = []
        self.n_issued = 0
        self.n_cons = 0
        self.released = 0

    def reset_for_real(self):
        self.n_issued = 0
        self.n_cons = 0
        self.released = 0

    def _issue(self):
        fw = self.fw
        while (self.n_issued < len(self.reqs) and self.n_issued <= self.n_cons + self.LOOK
               and self.n_issued - self.NSLOT < self.released):
            i = self.n_issued
            s = i % self.NSLOT
            src, n = self.reqs[i]
            fw.dma(fw.pool, f"w{s}", self.slots[s][:, 0:n, :], src, writes=[self.res[s]])
            self.n_issued += 1

    def get(self, src, n):
        if self.fw.dry:
            self.reqs.append((src, n))
            return self.slots[0], self.res[0]
        idx = self.n_cons
        self._issue()
        assert self.n_issued > idx, "weight stream discipline violated"
        self.n_cons += 1
        self._issue()
        s = idx % self.NSLOT
        return self.slots[s], self.res[s]

    def free(self):
        if self.fw.dry:
            return
        self.released = self.n_cons
        self._issue()


def wblock(w2d, r0, n, c0):
    return w2d[r0:r0 + 128 * n, c0:c0 + 128].rearrange("(i p) c -> p i c", p=128)


def _bucket(rel):
    nb = N_BUCKETS // 2
    max_exact = nb // 2
    n = np.abs(rel)
    large = max_exact + (np.log(np.maximum(n, 1).astype(np.float32) / max_exact)
                         / math.log(MAX_DISTANCE / max_exact) * (nb - max_exact)).astype(np.int32)
    large = np.minimum(large, nb - 1)
    return np.where(rel > 0, nb, 0) + np.where(n < max_exact, n, large)


def _bucket_onehot():
    m = np.arange(768)
    b = _bucket(127 - m)
    oh = np.zeros((32, 768), np.float32)
    oh[b, m] = 1.0
    return oh


class Builder:
    def __init__(self, n_seq, T, with_sample):
        self.n_seq, self.T, self.with_sample = n_seq, T, with_sample
        nc = self.nc = bass.Bass("TRN2", target_bir_lowering=False)
        self.fw = FW(nc)
        self._declare_io()
        self._alloc()

    def _declare_io(self):
        nc, n_seq, T = self.nc, self.n_seq, self.T

        def din(name, shape):
            return nc.dram_tensor(name, list(shape), F32, kind="ExternalInput").ap()

        def dout(name, shape):
            return nc.dram_tensor(name, list(shape), F32, kind="ExternalOutput").ap()

        self.xp = din("xp", [n_seq, 8, 128, T])
        self.xs = din("xs", [8, 128, 128])
        self.ckT = din("ckT", [2, 4, 8, 128, PAST])
        self.cv = din("cv", [2, 4, PAST, 8, 128])
        self.spT = din("spT", [2, 4, 4, 128, 15])
        self.cT = din("cT", [128, 64])
        self.relb = din("relb", [32, 8])
        self.ohm = din("ohm", [32, 768])
        self.w_ada = din("w_ada", [2, D, 6 * D])
        self.b_ada = din("b_ada_t", [128, 2, 48])
        self.w_in = din("w_in", [2, D, IN_W])
        self.b_gate = din("b_gate_t", [128, 2, 16])
        self.pool_w = din("pool_w", [2, 4, 128, 128])
        self.pscale = din("pscale_t", [128, 2, 4])
        self.lamqk = din("lamqk_b", [128, 2, 256])
        self.subln = din("subln_t", [128, 2])
        self.w_pool_up = din("w_pool_up", [2, 512, D])
        self.w_attn_up = din("w_attn_up", [2, D, D])
        self.w_o = din("w_o", [2, D, D])
        self.lng = din("lng_t", [128, 2, 2, 8])
        self.lnb = din("lnb_t", [128, 2, 2, 8])
        self.wfg = din("w_ffn_gate", [1, D, DFF])
        self.wfu = din("w_ffn_up", [1, D, DFF])
        self.wfd = din("w_ffn_down", [1, DFF, D])
        self.w_router = din("w_router_t", [128, 8, 8])
        self.weg = din("w_exp_gate", [1, NE, D, DFE])
        self.weu = din("w_exp_up", [1, NE, D, DFE])
        self.wed = din("w_exp_down", [1, NE, DFE, D])
        self.yp = dout("yp", [n_seq, T, D])
        self.ys = dout("ys", [128, D])
        self.nkp = dout("nkp", [2, n_seq, T, 8, 128])
        self.nvp = dout("nvp", [2, n_seq, T, 8, 128])
        self.npp = dout("npp", [2, n_seq, 15, 512])
        self.nks = dout("nks", [2, 4, 32, 8, 128])
        self.nvs = dout("nvs", [2, 4, 32, 8, 128])
        self.nps = dout("nps", [2, 4, 15, 512])
        self.gscr = nc.dram_tensor("gscr", [8, 128 * 768], F32, kind="Internal")
        self.wscr = nc.dram_tensor("wscr", [8, 128, 640], F32, kind="Internal")

    def _alloc(self):
        nc, T = self.nc, self.T
        sb = nc.alloc_sbuf_tensor
        self.X = sb("X", [128, 8, T], F32)
        self.Hh = sb("H", [128, 8, T], BF16)
        self.AO = sb("AO", [128, 8, T], BF16)
        self.PO = sb("PO", [128, 4, T], BF16)
        self.FA = sb("FA", [128, 4608], F32)
        self.BA = sb("BA", [128, 8192], BF16)
        self.RX = [Res(f"X{k}") for k in range(8)]
        self.RH = [Res(f"H{k}") for k in range(8)]
        self.RAO = [Res(f"AO{k}") for k in range(8)]
        self.RPO = [Res(f"PO{k}") for k in range(4)]
        self.ps = [nc.alloc_psum_tensor(f"ps{i}", [128, 512], F32) for i in range(8)]
        self.RP = [Res(f"ps{i}", excl=True) for i in range(8)]
        self.bank_rr = 0
        self.ones_mean = sb("ones_mean", [128, 128], F32)
        self.ones_rms = sb("ones_rms", [128, 128], F32)
        self.ones_f = sb("ones_f", [128, 128], F32)
        self.ones_b = sb("ones_b", [128, 128], BF16)
        self.ident = sb("ident", [128, 128], F32)
        self.eps = sb("eps", [128, 2], F32)
        self.t_bada = sb("t_bada", [128, 2, 48], F32)
        self.t_bgate = sb("t_bgate", [128, 2, 16], F32)
        self.t_pscale = sb("t_pscale", [128, 2, 4], F32)
        self.t_subln = sb("t_subln", [128, 2], F32)
        self.t_lng = sb("t_lng", [128, 2, 2, 8], F32)
        self.t_lnb = sb("t_lnb", [128, 2, 2, 8], F32)
        self.t_relb = sb("t_relb", [128, 8], F32)
        self.t_router = sb("t_router", [128, 8, 8], F32)
        self.MOD = sb("MOD", [128, 2, 48, 8], F32)
        self.SCA = sb("SCA", [128, 2, 2, 8, 8], F32)
        self.SCB = sb("SCB", [128, 2, 2, 8, 8], F32)
        self.LAMT = sb("LAMT", [128, 2, 8], F32)
        self.BF15 = sb("BF15", [128, 8], F32)
        self.INVC = sb("INVC", [128, 4, 16], F32)
        self.WH = [sb(f"WH{i}", [128, 640], F32) for i in range(2)]
        self.RWH = [Res(f"WH{i}") for i in range(2)]
        self.EX = [self.FA[:, 1024:1536], self.FA[:, 1536:2048]]
        self.REX = [Res(f"EX{i}") for i in range(2)]
        self.RC = Res("consts")
        self.ROUT = Res("outputs")
        self.RSCR = Res("dram_scratch")
        self.wst = WStream(self.fw, nc)

    def bank(self, allowed=None):
        allowed = allowed or range(8)
        allowed = list(allowed)
        i = allowed[self.bank_rr % len(allowed)]
        self.bank_rr += 1
        return i

    def mm(self, out, lhsT, rhs, start, stop):
        nc = self.nc
        return lambda: nc.tensor.matmul(out, lhsT, rhs, start=start, stop=stop)

    def tr(self, out, in_):
        nc = self.nc
        ident = self.ident
        return lambda: nc.tensor.transpose(out, in_, ident[:])

    def mm_acc(self, out, pairs, reads, writes):
        n = len(pairs)
        fns = [self.mm(out, l, r, i == 0, i == n - 1) for i, (l, r) in enumerate(pairs)]
        self.fw.mm_group(fns, reads, writes)

    def act(self, out, in_, func, reads, writes, bias=None, scale=None):
        nc = self.nc
        kw = {}
        if bias is not None:
            kw["bias"] = bias
        if scale is not None:
            kw["scale"] = scale
        self.fw.op(self.fw.act, lambda: nc.scalar.activation(out=out, in_=in_, func=func, **kw), reads, writes)

    def acopy(self, out, in_, reads, writes):
        nc = self.nc
        self.fw.op(self.fw.act, lambda: nc.scalar.copy(out, in_), reads, writes)

    def vcopy(self, out, in_, reads, writes):
        nc = self.nc
        self.fw.op(self.fw.dve, lambda: nc.vector.tensor_copy(out, in_), reads, writes)

    def vtt(self, out, in0, in1, op, reads, writes):
        nc = self.nc
        self.fw.op(self.fw.dve, lambda: nc.vector.tensor_tensor(out=out, in0=in0, in1=in1, op=op), reads, writes)

    def vts(self, out, in_, scalar, op, reads, writes):
        nc = self.nc
        self.fw.op(self.fw.dve, lambda: nc.vector.tensor_single_scalar(out=out, in_=in_, scalar=scalar, op=op),
                   reads, writes)

    def vts2(self, out, in_, s1, s2, op0, op1, reads, writes):
        nc = self.nc
        self.fw.op(self.fw.dve, lambda: nc.vector.tensor_scalar(out=out, in0=in_, scalar1=s1, scalar2=s2,
                                                                op0=op0, op1=op1), reads, writes)

    def vstt(self, out, in0, scalar, in1, op0, op1, reads, writes):
        nc = self.nc
        self.fw.op(self.fw.dve, lambda: nc.vector.scalar_tensor_tensor(out=out, in0=in0, scalar=scalar, in1=in1,
                                                                       op0=op0, op1=op1), reads, writes)

    def vmemset(self, ap, val, writes):
        nc = self.nc
        self.fw.op(self.fw.dve, lambda: nc.vector.memset(ap, val), (), writes)

    def dma_sp(self, key, out, in_, reads, writes):
        self.fw.dma(self.fw.sp, key, out, in_, reads, writes)

    def setup(self):
        nc, fw = self.nc, self.fw
        RC = self.RC
        FA = self.FA
        t_lamqk = FA[:, 128:640]
        self.vmemset(self.t_relb[:], 0.0, [RC])
        loads = [(self.t_bada, self.b_ada), (self.t_bgate, self.b_gate), (self.t_pscale, self.pscale),
                 (t_lamqk, self.lamqk.rearrange("p l n -> p (l n)")), (self.t_subln, self.subln),
                 (self.t_lng, self.lng), (self.t_lnb, self.lnb), (self.t_relb[0:32, :], self.relb),
                 (FA[:, 0:64], self.cT), (self.t_router, self.w_router)]
        for i, (t, src) in enumerate(loads):
            self.dma_sp(f"ld{i}", t[:] if not isinstance(t, bass.AP) else t, src, (), [RC])
        self.vmemset(self.ones_mean[:], 1.0 / 1024, [RC])
        self.vmemset(self.ones_rms[:], 1.0 / 128, [RC])
        self.vmemset(self.ones_f[:], 1.0, [RC])
        self.vmemset(self.ones_b[:], 1.0, [RC])
        self.vmemset(self.eps[:, 0:1], LN_EPS, [RC])
        self.vmemset(self.eps[:, 1:2], RMS_EPS, [RC])
        ident, INVC = self.ident, self.INVC
        fw.op(fw.pool, lambda: nc.gpsimd.memset(ident[:], 1.0), (), [RC])
        fw.op(fw.pool, lambda: nc.gpsimd.affine_select(out=ident[:], in_=ident[:], pattern=[[-1, 128]],
                                                       compare_op=ALU.is_equal, fill=0.0, base=0,
                                                       channel_multiplier=1), [RC], [RC])
        for g, w in enumerate(POOL_W):
            for q in range(16):
                self.vmemset(INVC[:, g, q:q + 1], 1.0 / min(q + 1, w), [RC])
        self.tick("s_consts")
        L = self.LAMT
        for l in range(2):
            tmp = FA[:, 640:704]
            RT = Res("tmp")
            for i in range(2):
                o = 256 * l + 128 * i
                self.vtt(tmp, t_lamqk[:, o:o + 64], t_lamqk[:, o + 64:o + 128], ALU.mult, [RC], [RT])
                fw.op(fw.dve, (lambda i=i, l=l: nc.vector.reduce_sum(out=L[:, l, 2 + i:3 + i], in_=tmp, axis=AX.X)),
                      [RT], [RC])
            self.act(L[:, l, 2:4], L[:, l, 2:4], AF.Exp, [RC], [RC])
            self.vtt(L[:, l, 0:1], L[:, l, 3:4], L[:, l, 2:3], ALU.subtract, [RC], [RC])
            self.vts(L[:, l, 0:1], L[:, l, 0:1], -LAM_INIT[l], ALU.add, [RC], [RC])
            self.vts(L[:, l, 1:2], self.t_subln[:, l:l + 1], 1.0 - LAM_INIT[l], ALU.mult, [RC], [RC])
        self.tick("s_lam")
        fw.barrier()
        scT = FA[:, 64:128]
        RS = Res("scT")
        self.act(scT, FA[:, 0:64], AF.Silu, [RC], [RS])
        wa = [FA[:, 512 + 1024 * i: 1536 + 1024 * i] for i in range(4)]
        RW = [Res(f"wa{i}") for i in range(4)]
        cnt = 0
        for l in range(2):
            for j in range(48):
                s = cnt % 4
                cnt += 1
                self.dma_sp(f"wa{s}", wa[s].rearrange("p (k c) -> p k c", k=8),
                            wblock(self.w_ada[l], 0, 8, j * 128), (), [RW[s]])
                b = self.bank()
                self.mm_acc(self.ps[b][:, 0:8],
                            [(wa[s][:, kc * 128:(kc + 1) * 128], scT[:, kc * 8:(kc + 1) * 8]) for kc in range(8)],
                            [RW[s], RS], [self.RP[b]])
                self.vts(self.MOD[:, l, j, :], self.ps[b][:, 0:8], self.t_bada[:, l, j:j + 1], ALU.add,
                         [self.RP[b], RC], [RC])
        self.tick("s_ada")
        M, SCA, SCB = self.MOD, self.SCA, self.SCB
        for l in range(2):
            for which in range(2):
                sh0 = 0 if which == 0 else 24
                sc0 = 8 if which == 0 else 32
                for kc in range(8):
                    a = SCA[:, l, which, kc, :]
                    bb = SCB[:, l, which, kc, :]
                    self.vts(a, M[:, l, sc0 + kc, :], 1.0, ALU.add, [RC], [RC])
                    if which == 0 and l == 0:
                        self.vcopy(bb, M[:, l, sh0 + kc, :], [RC], [RC])
                        continue
                    ll, wi = (l, 0) if which == 1 else (l - 1, 1)
                    g_ap = self.t_lng[:, ll, wi, kc:kc + 1]
                    b_ap = self.t_lnb[:, ll, wi, kc:kc + 1]
                    self.vstt(bb, a, b_ap, M[:, l, sh0 + kc, :], ALU.mult, ALU.add, [RC], [RC])
                    self.vts(a, a, g_ap, ALU.mult, [RC], [RC])
        self.tick("s_modtab")
        fw.barrier()
        RB = FA[:, 128:256]
        RRB = Res("RB")
        GT = FA[:, 256:1024]
        RG = Res("GT")
        WR = FA[:, 1024:1664]
        ALM = FA[:, 1664:2304]
        t_ohm = FA[:, 2304:3072]
        WRb = self.BA[:, 0:640]
        RWR = Res("WR")
        self.vmemset(t_ohm, 0.0, [RC])
        self.dma_sp("ld_ohm", FA[0:32, 2304:3072], self.ohm, (), [RC])
        self.vmemset(ALM, 1.0, [RC])
        self.vmemset(FA[64:128, 1664:1728], 0.0, [RC])
        self.tick("s_b0")
        import os
        for h in [int(c) for c in os.environ.get("K_HEADS", "01234567")]:
            if os.environ.get("K_E3"):
                self._e3 = getattr(self, "_e3", 0) + 1
                if self._e3 % 2 == 0:
                    GT = FA[:, 3072:3840]
                    RG = Res("GT2")
            it2 = getattr(self, "_it", 0)
            self._it = it2 + 1
            sub = int(os.environ.get("K_SUB", "99")) if it2 >= 1 else 99
            self.vts(RB, self.ones_f[:], self.t_relb[:, h:h + 1], ALU.mult, [RC], [RRB])
            if sub <= 1: raise StopIteration
            b0, b1 = self.bank(), self.bank()
            self.fw.mm_group([self.mm(self.ps[b0][:, 0:384], RB, t_ohm[:, 0:384], True, True),
                              self.mm(self.ps[b1][:, 0:384], RB, t_ohm[:, 384:768], True, True)],
                             [RRB, RC], [self.RP[b0], self.RP[b1]])
            if sub <= 2: raise StopIteration
            self.vcopy(self.BF15[:, h:h + 1], self.ps[b0][:, 300:301], [self.RP[b0]], [RC])
            if sub <= 3: raise StopIteration
            self.act(GT[:, 0:384], self.ps[b0][:, 0:384], AF.Exp, [self.RP[b0]], [RG])
            if sub <= 4: raise StopIteration
            self.act(GT[:, 384:768], self.ps[b1][:, 0:384], AF.Exp, [self.RP[b1]], [RG])
            if h <= 1: self.tick("s_b1")
            self.dma_sp("gscr", self.gscr.ap()[h].rearrange("(p n) -> p n", p=128), GT, [RG], [self.RSCR])
            src = bass.AP(tensor=self.gscr, offset=h * 128 * 768 + 127, ap=[[767, 128], [1, 640]])
            if h <= 1: self.tick("s_b2")
            self.dma_sp("toe", WR, src, [self.RSCR], [RWR])
            if h <= 1: self.tick("s_b3")
            self.vtt(WR, WR, ALM, ALU.mult, [RWR, RC], [RWR])
            if h <= 1: self.tick("s_b4")
            self.dma_sp("wscr", self.wscr.ap()[h], WR, [RWR], [self.RSCR])
            if h <= 1: self.tick("s_b5")
        fw.barrier()

    def make_group(self, kind, s):
        T = self.T if kind == "p" else 128
        TS = min(512, T)
        g = dict(kind=kind, s=s, T=T, TS=TS, NT=T // TS)
        if kind == "p":
            g["segs"] = [(0, T, s)]
        else:
            g["segs"] = [(32 * b, 32, 4 + b) for b in range(4)]
        return g

    def segs_in(self, g, t):
        TS = g["TS"]
        lo, hi = t * TS, (t + 1) * TS
        out = []
        for (s0, sl, bc) in g["segs"]:
            a, b = max(lo, s0), min(hi, s0 + sl)
            if a < b:
                out.append((a, b, bc))
        return out

    def modulate0(self, g):
        X, Hh = self.X, self.Hh
        for t in range(g["NT"]):
            for kc in range(8):
                for (a, b, bc) in self.segs_in(g, t):
                    self.act(Hh[:, kc, a:b], X[:, kc, a:b], AF.Identity, [self.RX[kc], self.RC], [self.RH[kc]],
                             bias=self.SCB[:, 0, 0, kc, bc:bc + 1], scale=self.SCA[:, 0, 0, kc, bc:bc + 1])

    def layernorm(self, g, l, which):
        fw = self.fw
        X, Hh, FA = self.X, self.Hh, self.FA
        TS = g["TS"]
        last = (l == 1 and which == 1)
        router = (l == 1 and which == 0)
        nl, nw = (l, 1) if which == 0 else (l + 1, 0)
        SQ = [FA[:, 0:512], FA[:, 512:1024]]
        RSQ = [Res("sq0"), Res("sq1")]
        MEAN = FA[:, 1024:1536]
        RSTD = FA[:, 1536:2048]
        TMP = [FA[:, 2048:2560], FA[:, 2560:3072]]
        HF = [FA[:, 3072:3584], FA[:, 3584:4096]]
        RM, RR = Res("mean"), Res("rstd")
        RT = [Res("t0"), Res("t1")]
        RHF = [Res("hf0"), Res("hf1")]
        for t in range(g["NT"]):
            c0, c1 = t * TS, (t + 1) * TS
            b1, b2 = self.bank(), self.bank()
            for kc in range(8):
                s = kc % 2
                self.vtt(SQ[s][:, 0:TS], X[:, kc, c0:c1], X[:, kc, c0:c1], ALU.mult, [self.RX[kc]], [RSQ[s]])
                self.fw.mm_group([self.mm(self.ps[b1][:, 0:TS], self.ones_mean[:], X[:, kc, c0:c1], kc == 0, kc == 7),
                                  self.mm(self.ps[b2][:, 0:TS], self.ones_mean[:], SQ[s][:, 0:TS], kc == 0, kc == 7)],
                                 [self.RX[kc], RSQ[s], self.RC], [self.RP[b1], self.RP[b2]])
            self.vcopy(MEAN[:, 0:TS], self.ps[b1][:, 0:TS], [self.RP[b1]], [RM])
            self.vtt(RSTD[:, 0:TS], MEAN[:, 0:TS], MEAN[:, 0:TS], ALU.mult, [RM], [RR])
            self.vtt(RSTD[:, 0:TS], self.ps[b2][:, 0:TS], RSTD[:, 0:TS], ALU.subtract, [self.RP[b2], RR], [RR])
            self.act(RSTD[:, 0:TS], RSTD[:, 0:TS], AF.Ln, [RR, self.RC], [RR], bias=self.eps[:, 0:1], scale=1.0)
            self.act(RSTD[:, 0:TS], RSTD[:, 0:TS], AF.Exp, [RR], [RR], scale=-0.5)
            if router:
                lb = self.bank()
            for kc in range(8):
                s = kc % 2
                self.vtt(TMP[s][:, 0:TS], X[:, kc, c0:c1], MEAN[:, 0:TS], ALU.subtract, [self.RX[kc], RM], [RT[s]])
                self.vtt(TMP[s][:, 0:TS], TMP[s][:, 0:TS], RSTD[:, 0:TS], ALU.mult, [RT[s], RR], [RT[s]])
                for (a, b, bc) in self.segs_in(g, t):
                    la, lb_ = a - c0, b - c0
                    self.act(X[:, kc, a:b], TMP[s][:, la:lb_], AF.Identity, [RT[s], self.RC], [self.RX[kc]],
                             bias=self.t_lnb[:, l, which, kc:kc + 1], scale=self.t_lng[:, l, which, kc:kc + 1])
                    if not last:
                        self.act(Hh[:, kc, a:b], TMP[s][:, la:lb_], AF.Identity, [RT[s], self.RC], [self.RH[kc]],
                                 bias=self.SCB[:, nl, nw, kc, bc:bc + 1], scale=self.SCA[:, nl, nw, kc, bc:bc + 1])
                    if router:
                        self.act(HF[s][:, la:lb_], TMP[s][:, la:lb_], AF.Identity, [RT[s], self.RC], [RHF[s]],
                                 bias=self.SCB[:, nl, nw, kc, bc:bc + 1], scale=self.SCA[:, nl, nw, kc, bc:bc + 1])
                if router:
                    nb = TS // 128
                    self.fw.mm_group([self.mm(self.ps[lb][:, 32 * kc + 8 * q:32 * kc + 8 * q + 8],
                                              HF[s][:, 128 * q:128 * q + 128], self.t_router[:, kc, :], True, True)
                                      for q in range(nb)],
                                     [RHF[s], self.RC], [self.RP[lb]])
            if router:
                self.route(g, t, lb)

    def route(self, g, t, lb):
        FA = self.FA
        TS = g["TS"]
        nb = TS // 128
        R = Res("route")
        LG = FA[:, 4096:4096 + 32]
        M1 = FA[:, 4128:4132]
        M2 = FA[:, 4132:4136]
        K1 = FA[:, 4136:4168]
        K2 = FA[:, 4168:4200]
        L2 = FA[:, 4200:4232]
        W1 = FA[:, 4232:4236]
        W2 = FA[:, 4236:4240]
        CB = FA[:, 4480 + t * nb * 8: 4480 + (t + 1) * nb * 8]
        nc, fw = self.nc, self.fw
        self.vcopy(LG[:, 0:8 * nb], self.ps[lb][:, 0:8 * nb], [self.RP[lb]], [R])
        for kc in range(1, 8):
            self.vtt(LG[:, 0:8 * nb], LG[:, 0:8 * nb], self.ps[lb][:, 32 * kc:32 * kc + 8 * nb], ALU.add,
                     [self.RP[lb], R], [R])
        for q in range(nb):
            lg = LG[:, 8 * q:8 * q + 8]
            fw.op(fw.dve, (lambda q=q, lg=lg: nc.vector.reduce_max(out=M1[:, q:q + 1], in_=lg, axis=AX.X)), [R], [R])
            self.vts(K1[:, 8 * q:8 * q + 8], lg, M1[:, q:q + 1], ALU.is_equal, [R], [R])
            self.vstt(L2[:, 8 * q:8 * q + 8], K1[:, 8 * q:8 * q + 8], -1e30, lg, ALU.mult, ALU.add, [R], [R])
            fw.op(fw.dve, (lambda q=q: nc.vector.reduce_max(out=M2[:, q:q + 1], in_=L2[:, 8 * q:8 * q + 8], axis=AX.X)),
                  [R], [R])
            self.vts(K2[:, 8 * q:8 * q + 8], L2[:, 8 * q:8 * q + 8], M2[:, q:q + 1], ALU.is_equal, [R], [R])
        self.vtt(W2[:, 0:nb], M2[:, 0:nb], M1[:, 0:nb], ALU.subtract, [R], [R])
        self.act(W2[:, 0:nb], W2[:, 0:nb], AF.Exp, [R], [R])
        self.vts(W1[:, 0:nb], W2[:, 0:nb], 1.0, ALU.add, [R], [R])
        fw.op(fw.dve, lambda: nc.vector.reciprocal(W1[:, 0:nb], W1[:, 0:nb]), [R], [R])
        self.vtt(W2[:, 0:nb], W2[:, 0:nb], W1[:, 0:nb], ALU.mult, [R], [R])
        for q in range(nb):
            self.vts(CB[:, 8 * q:8 * q + 8], K1[:, 8 * q:8 * q + 8], W1[:, q:q + 1], ALU.mult, [R], [self.RCB])
            self.vstt(CB[:, 8 * q:8 * q + 8], K2[:, 8 * q:8 * q + 8], W2[:, q:q + 1], CB[:, 8 * q:8 * q + 8],
                      ALU.mult, ALU.add, [R, self.RCB], [self.RCB])

    def proj_T(self, g, l, wsrc, nk, src_fn, src_res, evac):
        slot, rs = self.wst.get(wsrc, nk)
        TS = g["TS"]
        for t in range(g["NT"]):
            b = self.bank()
            self.mm_acc(self.ps[b][:, 0:TS], [(slot[:, k, :], src_fn(k, t)) for k in range(nk)],
                        [rs] + list(src_res), [self.RP[b]])
            evac(t, b)
        return slot, rs

    def pool_phase(self, g, l):
        fw, nc = self.fw, self.nc
        FA, BA, Hh, PO = self.FA, self.BA, self.Hh, self.PO
        T, TS, NT = g["T"], g["TS"], g["NT"]
        segs = g["segs"]
        nseg = len(segs)
        SL = segs[0][1]
        UW = 15 + SL
        U = FA[:, 0:nseg * UW]
        RU = Res("U")
        SA = FA[:, 2304:2304 + 15 + TS]
        SB_ = FA[:, 2880:2880 + 15 + TS]
        RSA, RSB = Res("SA"), Res("SB")
        UT = FA[:, 3456:3456 + 512]
        RUT = Res("UT")
        Dd = [BA[:, 0:512], BA[:, 512:1024]]
        RD = [Res("D0"), Res("D1")]
        w_l = self.w_in[l]
        for gi, w in enumerate(POOL_W):
            for si, (s0, sl, bc) in enumerate(segs):
                if g["kind"] == "p":
                    self.vmemset(U[:, si * UW: si * UW + 15], 0.0, [RU])
                else:
                    self.dma_sp("hist", U[:, si * UW: si * UW + 15], self.spT[l, si, gi], (), [RU])
            def evac(t, b, gi=gi):
                for (a, bnd, bc) in self.segs_in(g, t):
                    si = a // SL
                    off = si * UW + 15 + (a - si * SL)
                    self.acopy(U[:, off: off + (bnd - a)], self.ps[b][:, a - t * TS: bnd - t * TS],
                               [self.RP[b]], [RU])
            slot, rs = self.proj_T(g, l, wblock(w_l, 0, 8, gi * 128), 8,
                                   lambda k, t: Hh[:, k, t * TS:(t + 1) * TS], self.RH, evac)
            b = self.bank()
            self.mm_acc(self.ps[b][:, 0:128], [(Hh[:, k, T - 128:T], slot[:, k, :]) for k in range(8)],
                        [rs] + self.RH, [self.RP[b]])
            self.vcopy(UT[:, gi * 128:(gi + 1) * 128], self.ps[b][:, 0:128], [self.RP[b]], [RUT])
            self.wst.free()
            pw, rpw = self.wst.get(self.pool_w[l, gi].rearrange("(i p) c -> p i c", p=128), 1)
            for si, (s0, sl, bc) in enumerate(segs):
                nsub = max(1, sl // TS)
                L = min(sl, TS)
                for tt in range(nsub):
                    base = si * UW + tt * TS
                    FV = U[:, base: base + 15 + L]
                    Wd = 15 + L
                    cur, rcur = FV, RU
                    bufs = [(SA, RSA), (SB_, RSB)]
                    sh = 1
                    for m in range(gi + 1):
                        lo = 2 * sh - 1
                        dst, rdst = bufs[m % 2]
                        self.vtt(dst[:, lo:Wd], cur[:, lo:Wd], cur[:, lo - sh:Wd - sh], ALU.add, [rcur], [rdst])
                        cur, rcur = dst, rdst
                        sh *= 2
                    di = (tt + si) % 2
                    dd = Dd[di][:, 0:L]
                    self.vstt(dd, cur[:, 15:Wd], 1.0 / w, FV[:, 15:Wd], ALU.mult, ALU.subtract, [rcur, RU], [RD[di]])
                    if g["kind"] == "p" and tt == 0:
                        tmp = SB_[:, 0:16] if cur is SA else SA[:, 0:16]
                        rtmp = RSB if cur is SA else RSA
                        self.vtt(tmp, cur[:, 15:31], self.INVC[:, gi, :], ALU.mult, [rcur, self.RC], [rtmp])
                        self.vtt(dd[:, 0:16], tmp, FV[:, 15:31], ALU.subtract, [rtmp, RU], [RD[di]])
                    b = self.bank()
                    self.mm_acc(self.ps[b][:, 0:L], [(pw[:, 0, :], dd)], [rpw, RD[di]], [self.RP[b]])
                    c0 = s0 + tt * TS
                    self.act(PO[:, gi, c0:c0 + L], self.ps[b][:, 0:L], AF.Copy, [self.RP[b], self.RC], [self.RPO[gi]],
                             scale=self.t_pscale[:, l, gi:gi + 1])
            self.wst.free()
        if g["kind"] == "p":
            self.dma_sp("o_np", self.npp[l, g["s"]], UT[113:128, :], [RUT], ())
        else:
            for b in range(4):
                self.dma_sp("o_np", self.nps[l, b], UT[32 * b + 17:32 * b + 32, :], [RUT], ())

    def attn_finalize(self, g, l, h, c0, n, accb, sbank):
        FA = self.FA
        F = [FA[:, 2048 + 512 * i: 2048 + 512 * i + n] for i in range(4)]
        R = [Res(f"fin{i}") for i in range(4)]
        o0b, o1b, z0b, z1b = accb
        nc, fw = self.nc, self.fw
        fw.op(fw.dve, lambda: nc.vector.reciprocal(F[0], self.ps[z0b][:, 0:n]), [self.RP[z0b]], [R[0]])
        fw.op(fw.dve, lambda: nc.vector.reciprocal(F[1], self.ps[z1b][:, 0:n]), [self.RP[z1b]], [R[1]])
        self.vtt(F[0], self.ps[o0b][:, 0:n], F[0], ALU.mult, [self.RP[o0b], R[0]], [R[0]])
        self.vtt(F[1], self.ps[o1b][:, 0:n], F[1], ALU.mult, [self.RP[o1b], R[1]], [R[1]])
        self.vstt(F[2], F[1], self.LAMT[:, l, 0:1], F[0], ALU.mult, ALU.add, [R[0], R[1], self.RC], [R[2]])
        self.vtt(F[3], F[2], F[2], ALU.mult, [R[2]], [R[3]])
        self.mm_acc(self.ps[sbank][:, 0:n], [(self.ones_rms[:], F[3])], [R[3], self.RC], [self.RP[sbank]])
        self.act(F[0], self.ps[sbank][:, 0:n], AF.Ln, [self.RP[sbank], self.RC], [R[0]], bias=self.eps[:, 1:2], scale=1.0)
        self.act(F[0], F[0], AF.Exp, [R[0]], [R[0]], scale=-0.5)
        self.vstt(self.AO[:, h, c0:c0 + n], F[2], self.LAMT[:, l, 1:2], F[0], ALU.mult, ALU.mult,
                  [R[2], R[0], self.RC], [self.RAO[h]])

    def attn_prompt(self, g, l):
        fw, nc = self.fw, self.nc
        FA, BA, Hh = self.FA, self.BA, self.Hh
        T, TS, NT, s = g["T"], g["TS"], g["NT"], g["s"]
        NB = T // 128
        QT = BA[:, 0:T]
        KT = BA[:, 2048:2048 + T]
        V = BA[:, 4096:4096 + T]
        RQ, RK, RV = Res("QT"), Res("KT"), Res("V")
        E = [BA[:, 6144 + 512 * i: 6144 + 512 * (i + 1)] for i in range(4)]
        RE = [Res(f"E{i}") for i in range(4)]
        KST = [FA[:, 0:512], FA[:, 0:512]]
        VST = [FA[:, 512:1024], FA[:, 512:1024]]
        _rk, _rv = Res("kst"), Res("vst")
        RKS = [_rk, _rk]
        RVS = [_rv, _rv]
        w_l = self.w_in[l]
        ecnt = 0
        for h in range(NH):
            wi = h % 2
            self.dma_sp(f"wh{wi}", self.WH[wi][:], self.wscr.ap()[h], [self.RSCR], [self.RWH[wi]])
            WHh, RWHh = self.WH[wi], self.RWH[wi]
            self.chk("wh")
            self.proj_T(g, l, wblock(w_l, 0, 8, O_Q + h * 128), 8, lambda k, t: Hh[:, k, t * TS:(t + 1) * TS], self.RH,
                        lambda t, b: self.acopy(QT[:, t * TS:(t + 1) * TS], self.ps[b][:, 0:TS], [self.RP[b]], [RQ]))
            kslot, krs = self.proj_T(g, l, wblock(w_l, 0, 8, O_K + h * 128), 8,
                                     lambda k, t: Hh[:, k, t * TS:(t + 1) * TS], self.RH,
                                     lambda t, b: self.acopy(KT[:, t * TS:(t + 1) * TS], self.ps[b][:, 0:TS],
                                                             [self.RP[b]], [RK]))
            self.chk("qk")
            for q4 in range(NB // 4):
                b = self.bank()
                for qq in range(4):
                    blk = q4 * 4 + qq
                    self.mm_acc(self.ps[b][:, 128 * qq:128 * qq + 128],
                                [(Hh[:, k, blk * 128:(blk + 1) * 128], kslot[:, k, :]) for k in range(8)],
                                [krs] + self.RH, [self.RP[b]])
                si = q4 % 2
                self.vcopy(KST[si], self.ps[b][:, 0:512], [self.RP[b]], [RKS[si]])
                self.chk("ktok")
                self.dma_sp(f"o_k{si}", self.nkp[l, s, q4 * 512:(q4 + 1) * 512, h, :].rearrange("(b p) d -> p b d", p=128),
                            KST[si].rearrange("p (b d) -> p b d", b=4), [RKS[si]], ())
            self.chk("kdma")
            vslot, vrs = self.wst.get(wblock(w_l, 0, 8, O_V + h * 128), 8)
            for q4 in range(NB // 4):
                b = self.bank()
                for qq in range(4):
                    blk = q4 * 4 + qq
                    self.mm_acc(self.ps[b][:, 128 * qq:128 * qq + 128],
                                [(Hh[:, k, blk * 128:(blk + 1) * 128], vslot[:, k, :]) for k in range(8)],
                                [vrs] + self.RH, [self.RP[b]])
                si = q4 % 2
                self.acopy(V[:, q4 * 512:(q4 + 1) * 512], self.ps[b][:, 0:512], [self.RP[b]], [RV])
                self.vcopy(VST[si], self.ps[b][:, 0:512], [self.RP[b]], [RVS[si]])
                self.dma_sp(f"o_v{si}", self.nvp[l, s, q4 * 512:(q4 + 1) * 512, h, :].rearrange("(b p) d -> p b d", p=128),
                            VST[si].rearrange("p (b d) -> p b d", b=4), [RVS[si]], ())
            self.wst.free()
            self.chk("v")
            for t in range(NT):
                nbq = TS // 128
                jmax = nbq * t + nbq - 1

                def emit_S(j, par):
                    dl = j - nbq * t
                    q0 = max(0, 128 * dl)
                    sb0, sb1 = (4, 5) if par == 0 else (6, 7)
                    qs = t * TS + q0
                    self.fw.mm_group(
                        [self.mm(self.ps[sb0][:, q0:TS], KT[0:64, j * 128:(j + 1) * 128], QT[0:64, qs:(t + 1) * TS], True, True),
                         self.mm(self.ps[sb1][:, q0:TS], KT[64:128, j * 128:(j + 1) * 128], QT[64:128, qs:(t + 1) * TS], True, True)],
                        [RK, RQ], [self.RP[sb0], self.RP[sb1]])

                def emit_E(j, par):
                    dl = j - nbq * t
                    q0 = max(0, 128 * dl)
                    sb0, sb1 = (4, 5) if par == 0 else (6, 7)
                    e0, e1 = (0, 1) if par == 0 else (2, 3)
                    far = dl <= -2
                    for (sbk, ei) in ((sb0, e0), (sb1, e1)):
                        if far:
                            self.act(E[ei][:, q0:TS], self.ps[sbk][:, q0:TS], AF.Exp, [self.RP[sbk], self.RC], [RE[ei]],
                                     bias=self.BF15[:, h:h + 1], scale=0.125)
                        else:
                            xi = ei % 2
                            self.act(self.EX[xi][:, q0:TS], self.ps[sbk][:, q0:TS], AF.Exp, [self.RP[sbk]], [self.REX[xi]], scale=0.125)
                            self.vtt(E[ei][:, q0:TS], self.EX[xi][:, q0:TS], WHh[:, q0 - 128 * dl: TS - 128 * dl], ALU.mult,
                                     [self.REX[xi], RWHh], [RE[ei]])

                def emit_AV(j, par):
                    dl = j - nbq * t
                    q0 = max(0, 128 * dl)
                    e0, e1 = (0, 1) if par == 0 else (2, 3)
                    st, sp_ = (j == 0), (j == jmax)
                    self.fw.mm_group(
                        [self.mm(self.ps[0][:, q0:TS], V[:, j * 128:(j + 1) * 128], E[e0][:, q0:TS], st, sp_),
                         self.mm(self.ps[2][:, q0:TS], self.ones_b[:], E[e0][:, q0:TS], st, sp_),
                         self.mm(self.ps[1][:, q0:TS], V[:, j * 128:(j + 1) * 128], E[e1][:, q0:TS], st, sp_),
                         self.mm(self.ps[3][:, q0:TS], self.ones_b[:], E[e1][:, q0:TS], st, sp_)],
                        [RV, RE[e0], RE[e1], self.RC], [self.RP[0], self.RP[1], self.RP[2], self.RP[3]])

                emit_S(0, ecnt % 2)
                for j in range(jmax + 1):
                    par = (ecnt + j) % 2
                    emit_E(j, par)
                    if j < jmax:
                        emit_S(j + 1, 1 - par)
                    emit_AV(j, par)
                ecnt += jmax + 1
                sbank = 4 if (ecnt % 2 == 0) else 6
                self.attn_finalize(g, l, h, t * TS, TS, (0, 1, 2, 3), sbank)

    def attn_sample(self, g, l):
        fw, nc = self.fw, self.nc
        FA, BA, Hh = self.FA, self.BA, self.Hh
        QT = BA[:, 0:128]
        KT = BA[:, 128:256]
        RQ, RK = Res("QT"), Res("KT")
        VN = [BA[0:32, 256 + 128 * b: 384 + 128 * b] for b in range(4)]
        RVN = Res("VN")
        E = [BA[:, 1024 + 512 * i: 1024 + 512 * (i + 1)] for i in range(4)]
        RE = [Res(f"E{i}") for i in range(4)]
        KST = FA[0:32, 0:512]
        VST = FA[0:32, 512:1024]
        RKS, RVS = Res("kst"), Res("vst")
        w_l = self.w_in[l]
        ecnt = 0
        for h in range(NH):
            wi = h % 2
            self.dma_sp(f"wh{wi}", self.WH[wi][:], self.wscr.ap()[h], [self.RSCR], [self.RWH[wi]])
            WHh, RWHh = self.WH[wi], self.RWH[wi]
            self.proj_T(g, l, wblock(w_l, 0, 8, O_Q + h * 128), 8, lambda k, t: Hh[:, k, 0:128], self.RH,
                        lambda t, b: self.acopy(QT, self.ps[b][:, 0:128], [self.RP[b]], [RQ]))
            kslot, krs = self.proj_T(g, l, wblock(w_l, 0, 8, O_K + h * 128), 8, lambda k, t: Hh[:, k, 0:128], self.RH,
                                     lambda t, b: self.acopy(KT, self.ps[b][:, 0:128], [self.RP[b]], [RK]))
            b = self.bank()
            for bb in range(4):
                self.mm_acc(self.ps[b][0:32, 128 * bb:128 * bb + 128],
                            [(Hh[:, k, 32 * bb:32 * bb + 32], kslot[:, k, :]) for k in range(8)],
                            [krs] + self.RH, [self.RP[b]])
            self.vcopy(KST, self.ps[b][0:32, 0:512], [self.RP[b]], [RKS])
            for bb in range(4):
                self.dma_sp("o_k0", self.nks[l, bb, :, h, :], KST[:, 128 * bb:128 * bb + 128], [RKS], ())
            vslot, vrs = self.wst.get(wblock(w_l, 0, 8, O_V + h * 128), 8)
            b = self.bank()
            for bb in range(4):
                self.mm_acc(self.ps[b][0:32, 128 * bb:128 * bb + 128],
                            [(Hh[:, k, 32 * bb:32 * bb + 32], vslot[:, k, :]) for k in range(8)],
                            [vrs] + self.RH, [self.RP[b]])
            self.acopy(BA[0:32, 256:768], self.ps[b][0:32, 0:512], [self.RP[b]], [RVN])
            self.vcopy(VST, self.ps[b][0:32, 0:512], [self.RP[b]], [RVS])
            for bb in range(4):
                self.dma_sp("o_v0", self.nvs[l, bb, :, h, :], VST[:, 128 * bb:128 * bb + 128], [RVS], ())
            self.wst.free()
            for bb in range(4):
                kc_slot, kcr = self.wst.get(self.ckT[l, bb, h].rearrange("p (a b) -> p a b", b=128), 8)
                vc_slot, vcr = self.wst.get(self.cv[l, bb, :, h, :].rearrange("(j p) d -> p j d", p=128), 8)
                sb0, sb1 = (4, 5) if (ecnt % 2 == 0) else (6, 7)
                e0, e1 = (0, 1) if (ecnt % 2 == 0) else (2, 3)
                ecnt += 1
                qcol = slice(32 * bb, 32 * bb + 32)
                fns = []
                for j in range(8):
                    fns.append(self.mm(self.ps[sb0][:, 32 * j:32 * j + 32], kc_slot[0:64, j, :], QT[0:64, qcol], True, True))
                    fns.append(self.mm(self.ps[sb1][:, 32 * j:32 * j + 32], kc_slot[64:128, j, :], QT[64:128, qcol], True, True))
                fns.append(self.mm(self.ps[sb0][0:32, 256:288], KT[0:64, qcol], QT[0:64, qcol], True, True))
                fns.append(self.mm(self.ps[sb1][0:32, 256:288], KT[64:128, qcol], QT[64:128, qcol], True, True))
                self.fw.mm_group(fns, [kcr, RQ, RK], [self.RP[sb0], self.RP[sb1]])
                for (sbk, ei) in ((sb0, e0), (sb1, e1)):
                    self.act(E[ei][:, 0:224], self.ps[sbk][:, 0:224], AF.Exp, [self.RP[sbk], self.RC], [RE[ei]],
                             bias=self.BF15[:, h:h + 1], scale=0.125)
                    xi = ei % 2
                    EXt = self.EX[xi]
                    self.act(EXt[:, 224:256], self.ps[sbk][:, 224:256], AF.Exp, [self.RP[sbk]], [self.REX[xi]], scale=0.125)
                    self.act(EXt[0:32, 256:288], self.ps[sbk][0:32, 256:288], AF.Exp, [self.RP[sbk]], [self.REX[xi]], scale=0.125)
                    self.vtt(E[ei][:, 224:256], EXt[:, 224:256], WHh[:, 128:160], ALU.mult, [self.REX[xi], RWHh], [RE[ei]])
                    self.vtt(E[ei][0:32, 256:288], EXt[0:32, 256:288], WHh[0:32, 0:32], ALU.mult, [self.REX[xi], RWHh], [RE[ei]])
                fns = []
                for ci, (ob, zb, ei) in enumerate(((0, 2, e0), (1, 3, e1))):
                    for j in range(8):
                        fns.append(self.mm(self.ps[ob][:, qcol], vc_slot[:, j, :], E[ei][:, 32 * j:32 * j + 32], j == 0, False))
                    fns.append(self.mm(self.ps[ob][:, qcol], VN[bb], E[ei][0:32, 256:288], False, True))
                    for j in range(8):
                        fns.append(self.mm(self.ps[zb][:, qcol], self.ones_b[:], E[ei][:, 32 * j:32 * j + 32], j == 0, False))
                    fns.append(self.mm(self.ps[zb][:, qcol], self.ones_b[0:32, :], E[ei][0:32, 256:288], False, True))
                self.fw.mm_group(fns, [vcr, RVN, RE[e0], RE[e1], self.RC], [self.RP[0], self.RP[1], self.RP[2], self.RP[3]])
                self.wst.free()
            sbank = 4 if (ecnt % 2 == 0) else 6
            self.attn_finalize(g, l, h, 0, 128, (0, 1, 2, 3), sbank)

    def merge_phase(self, g, l):
        FA, BA, Hh, AO, PO, X = self.FA, self.BA, self.Hh, self.AO, self.PO, self.X
        T, TS, NT = g["T"], g["TS"], g["NT"]
        w_l = self.w_in[l]
        halves = [(0, T)] if T <= 1024 else [(0, T // 2), (T // 2, T)]
        TM = [FA[:, 512 * i: 512 * (i + 1)] for i in range(4)]
        RTM = [Res(f"tm{i}") for i in range(4)]
        RMg = [Res(f"M{k}") for k in range(8)]
        for (h0, h1) in halves:
            HL = h1 - h0
            tiles = [(c, min(c + TS, h1)) for c in range(h0, h1, TS)]

            def Mv(kc, a, b):
                return BA[:, kc * HL + (a - h0): kc * HL + (b - h0)]
            cnt = 0
            for j in range(8):
                gp, rgp = self.wst.get(wblock(w_l, 0, 8, O_GP + j * 128), 8)
                ga, rga = self.wst.get(wblock(w_l, 0, 8, O_GA + j * 128), 8)
                pu, rpu = self.wst.get(wblock(self.w_pool_up[l], 0, 4, j * 128), 4)
                au, rau = self.wst.get(wblock(self.w_attn_up[l], 0, 8, j * 128), 8)
                for (a, b) in tiles:
                    n = b - a
                    b1, b2, b3, b4 = self.bank(), self.bank(), self.bank(), self.bank()
                    self.mm_acc(self.ps[b1][:, 0:n], [(gp[:, k, :], Hh[:, k, a:b]) for k in range(8)], [rgp] + self.RH, [self.RP[b1]])
                    self.mm_acc(self.ps[b2][:, 0:n], [(ga[:, k, :], Hh[:, k, a:b]) for k in range(8)], [rga] + self.RH, [self.RP[b2]])
                    self.mm_acc(self.ps[b3][:, 0:n], [(pu[:, k, :], PO[:, k, a:b]) for k in range(4)], [rpu] + self.RPO, [self.RP[b3]])
                    self.mm_acc(self.ps[b4][:, 0:n], [(au[:, k, :], AO[:, k, a:b]) for k in range(8)], [rau] + self.RAO, [self.RP[b4]])
                    i0 = (cnt % 2) * 2
                    cnt += 1
                    t1, t2 = TM[i0][:, 0:n], TM[i0 + 1][:, 0:n]
                    r1, r2 = RTM[i0], RTM[i0 + 1]
                    self.act(t1, self.ps[b1][:, 0:n], AF.Sigmoid, [self.RP[b1], self.RC], [r1], bias=self.t_bgate[:, l, j:j + 1], scale=1.0)
                    self.act(t2, self.ps[b2][:, 0:n], AF.Sigmoid, [self.RP[b2], self.RC], [r2], bias=self.t_bgate[:, l, 8 + j:9 + j], scale=1.0)
                    self.vtt(t1, self.ps[b3][:, 0:n], t1, ALU.mult, [self.RP[b3], r1], [r1])
                    self.vtt(t2, self.ps[b4][:, 0:n], t2, ALU.mult, [self.RP[b4], r2], [r2])
                    self.vtt(Mv(j, a, b), t1, t2, ALU.add, [r1, r2], [RMg[j]])
                self.wst.free()
            for j in range(8):
                wo, rwo = self.wst.get(wblock(self.w_o[l], 0, 8, j * 128), 8)
                for (a, b) in tiles:
                    n = b - a
                    bk = self.bank()
                    self.mm_acc(self.ps[bk][:, 0:n], [(wo[:, k, :], Mv(k, a, b)) for k in range(8)], [rwo] + RMg, [self.RP[bk]])
                    self.x_update(g, l, 0, j, a, b, bk, first=True)
                self.wst.free()
            self.fw.barrier()

    def x_update(self, g, l, which, j, a, b, bk, first):
        gate0 = 16 if which == 0 else 40
        for (s0, sl, bc) in g["segs"]:
            lo, hi = max(a, s0), min(b, s0 + sl)
            if lo >= hi:
                continue
            gate = self.MOD[:, l, gate0 + j, bc:bc + 1]
            pv = self.ps[bk][:, lo - a:hi - a]
            xv = self.X[:, j, lo:hi]
            if first:
                tmpi = self.xu_cnt % 2
                self.xu_cnt += 1
                tmp = self.FA[:, 3072 + 512 * tmpi: 3072 + 512 * tmpi + (hi - lo)]
                rt = self.RXU[tmpi]
                self.act(tmp, pv, AF.Copy, [self.RP[bk], self.RC], [rt], scale=gate)
                self.vstt(xv, xv, ALPHA, tmp, ALU.mult, ALU.add, [self.RX[j], rt], [self.RX[j]])
            else:
                self.vstt(xv, pv, gate, xv, ALU.mult, ALU.add, [self.RP[bk], self.RX[j], self.RC], [self.RX[j]])

    def ffn_phase(self, g, l):
        FA, BA, Hh, AO, X = self.FA, self.BA, self.Hh, self.AO, self.X
        T, TS, NT = g["T"], g["TS"], g["NT"]
        if l == 0:
            experts = [(self.wfg[0], self.wfu[0], self.wfd[0], DFF // 128, None)]
        else:
            experts = [(self.weg[0, e], self.weu[0, e], self.wed[0, e], DFE // 128, e) for e in range(NE)]
        SG = [FA[:, 0:512], FA[:, 512:1024]]
        RSG = [Res("sg0"), Res("sg1")]
        BC = FA[:, 1024:1024 + T]
        RBC = Res("BC")
        DG = [FA[:, 4096:4224], FA[:, 4224:4352]]
        RDG = [Res("dg0"), Res("dg1")]
        RA = [Res(f"A{k}") for k in range(8)]
        first = True
        cnt = 0
        for (wg, wu, wd, nff, e) in experts:
            if e is not None:
                for q4 in range(max(1, T // 512)):
                    bk = self.bank()
                    nq = min(4, T // 128)
                    for qq in range(nq):
                        blk = q4 * 4 + qq
                        di = blk % 2
                        self.vts(DG[di], self.ident[:], self.CBall[:, blk * 8 + e: blk * 8 + e + 1], ALU.mult,
                                 [self.RC, self.RCB], [RDG[di]])
                        self.mm_acc(self.ps[bk][:, 128 * qq:128 * qq + 128], [(self.ones_f[:], DG[di])],
                                    [self.RC, RDG[di]], [self.RP[bk]])
                    self.vcopy(BC[:, q4 * 512: q4 * 512 + 128 * nq], self.ps[bk][:, 0:128 * nq], [self.RP[bk]], [RBC])
            ngrp = (nff + 7) // 8
            base, rem = nff // ngrp, nff % ngrp
            i0 = 0
            for gi in range(ngrp):
                gsz = base + (1 if gi < rem else 0)
                for il in range(gsz):
                    i = i0 + il
                    sg_, rsg = self.wst.get(wblock(wg, 0, 8, i * 128), 8)
                    su_, rsu = self.wst.get(wblock(wu, 0, 8, i * 128), 8)
                    for t in range(NT):
                        c0, c1 = t * TS, (t + 1) * TS
                        b1, b2 = self.bank(), self.bank()
                        self.mm_acc(self.ps[b1][:, 0:TS], [(sg_[:, k, :], Hh[:, k, c0:c1]) for k in range(8)], [rsg] + self.RH, [self.RP[b1]])
                        self.mm_acc(self.ps[b2][:, 0:TS], [(su_[:, k, :], Hh[:, k, c0:c1]) for k in range(8)], [rsu] + self.RH, [self.RP[b2]])
                        si = cnt % 2
                        cnt += 1
                        s_, rs_ = SG[si][:, 0:TS], RSG[si]
                        self.act(s_, self.ps[b1][:, 0:TS], AF.Silu, [self.RP[b1]], [rs_])
                        if e is None:
                            self.vtt(AO[:, il, c0:c1], s_, self.ps[b2][:, 0:TS], ALU.mult, [rs_, self.RP[b2]], [RA[il]])
                        else:
                            self.vtt(s_, s_, self.ps[b2][:, 0:TS], ALU.mult, [rs_, self.RP[b2]], [rs_])
                            self.vtt(AO[:, il, c0:c1], s_, BC[:, c0:c1], ALU.mult, [rs_, RBC], [RA[il]])
                    self.wst.free()
                for j in range(8):
                    sd_, rsd = self.wst.get(wblock(wd, i0 * 128, gsz, j * 128), gsz)
                    for t in range(NT):
                        c0, c1 = t * TS, (t + 1) * TS
                        bk = self.bank()
                        self.mm_acc(self.ps[bk][:, 0:TS], [(sd_[:, il, :], AO[:, il, c0:c1]) for il in range(gsz)],
                                    [rsd] + RA[0:gsz], [self.RP[bk]])
                        self.x_update(g, l, 1, j, c0, c1, bk, first=first)
                    self.wst.free()
                first = False
                i0 += gsz

    def write_y(self, g):
        FA, X = self.FA, self.X
        nc = self.nc
        T = g["T"]
        YS = [FA[:, 0:1024], FA[:, 1024:2048]]
        RY = [Res("ys0"), Res("ys1")]
        for blk in range(T // 128):
            si = blk % 2
            for half in range(2):
                bk = self.bank()
                self.fw.mm_group([self.tr(self.ps[bk][:, 128 * (kc % 4):128 * (kc % 4) + 128],
                                          X[:, kc, blk * 128:(blk + 1) * 128])
                                  for kc in range(4 * half, 4 * half + 4)],
                                 self.RX[4 * half:4 * half + 4] + [self.RC], [self.RP[bk]])
                if half == 0:
                    self.vcopy(YS[si][:, 0:512], self.ps[bk][:, 0:512], [self.RP[bk]], [RY[si]])
                else:
                    self.acopy(YS[si][:, 512:1024], self.ps[bk][:, 0:512], [self.RP[bk]], [RY[si]])
            if g["kind"] == "p":
                dst = self.yp[g["s"], blk * 128:(blk + 1) * 128, :]
            else:
                dst = self.ys
            self.dma_sp(f"o_y{si}", dst, YS[si], [RY[si]], ())

    def run_group(self, g):
        fw = self.fw
        X = self.X
        T = g["T"]
        self.CBall = self.FA[:, 4480:4608]
        self.RCB = Res("CB")
        self.xu_cnt = 0
        self.RXU = [Res("xu0"), Res("xu1")]
        src = self.xp[g["s"]] if g["kind"] == "p" else self.xs
        if g["kind"] == "p":
            for kc in range(8):
                self.dma_sp(f"ldx{kc}", X[:, kc, :], src[kc], (), [self.RX[kc]])
        else:
            for kc in range(8):
                self.dma_sp(f"ldx{kc}", X[:, kc, 0:128], src[kc], (), [self.RX[kc]])
        self.modulate0(g)
        self.tick("mod0")
        for l in range(2):
            fw.barrier()
            self.pool_phase(g, l)
            self.tick("pool")
            fw.barrier()
            if g["kind"] == "p":
                self.attn_prompt(g, l)
            else:
                self.attn_sample(g, l)
            self.tick("attn")
            fw.barrier()
            self.merge_phase(g, l)
            self.tick("merge")
            fw.barrier()
            self.layernorm(g, l, 0)
            self.tick("ln0")
            fw.barrier()
            self.ffn_phase(g, l)
            self.tick("ffn")
            fw.barrier()
            self.layernorm(g, l, 1)
            self.tick("ln1")
            fw.barrier()
        self.write_y(g)
        self.tick("y")
        fw.barrier()

    def body(self):
        import os
        self.bank_rr = 0
        self._it = 0
        self._chk = 0
        self.phase_no = 0
        self.stop_at = int(os.environ.get("K_STOP", "100000"))
        try:
            self.setup()
            self.tick("setup")
            order = os.environ.get("K_ORDER", "ps")
            for ch in order:
                if ch == "p":
                    for s in range(self.n_seq):
                        self.run_group(self.make_group("p", s))
                elif self.with_sample:
                    self.run_group(self.make_group("s", 0))
        except StopIteration:
            pass

    def chk(self, name=""):
        self._chk = getattr(self, "_chk", 0) + 1
        lim = int(os.environ.get("K_CHK", "0"))
        if lim and self._chk >= lim:
            if not self.fw.dry:
                print("CHK stop at", self._chk, name)
            raise StopIteration

    def tick(self, name):
        self.phase_no += 1
        if not self.fw.dry and os.environ.get("K_VERBOSE"):
            print("phase", self.phase_no, name, "ins", self.fw.n_ins, "waits", self.fw.n_wait)
        if self.phase_no >= self.stop_at:
            raise StopIteration

    def build(self):
        fw = self.fw
        fw.dry = True
        self.body()
        fw.dry = False
        self.wst.reset_for_real()
        self.body()
        fw.barrier(engines=("sp",))
        fw.flush()
        return self.nc


def _tab(v, n):
    return np.ascontiguousarray(np.asarray(v, np.float32).reshape(n, 128).T)


def prep_shared(inp):
    f = lambda a: np.ascontiguousarray(np.asarray(a, np.float32))
    sh = {}
    sh["relb"] = f(inp["rel_bias"])
    sh["ohm"] = _bucket_onehot()
    sh["w_ada"] = f(inp["w_ada"])
    sh["b_ada_t"] = np.ascontiguousarray(np.stack([_tab(inp["b_ada"][l], 48) for l in range(2)], axis=1))
    sh["w_in"] = f(inp["w_in"])
    sh["b_gate_t"] = np.ascontiguousarray(np.stack([_tab(inp["b_gate"][l], 16) for l in range(2)], axis=1))
    sh["pool_w"] = f(inp["pool_w"])
    sh["pscale_t"] = np.ascontiguousarray(np.stack([_tab(inp["pool_scale"][l], 4) for l in range(2)], axis=1))
    lq = np.asarray(inp["lam_qk"], np.float32).reshape(1, 2, 256)
    sh["lamqk_b"] = np.ascontiguousarray(np.broadcast_to(lq, (128, 2, 256)))
    sh["subln_t"] = np.ascontiguousarray(np.asarray(inp["subln_g"], np.float32).T)
    sh["w_pool_up"] = f(inp["w_pool_up"])
    sh["w_attn_up"] = f(inp["w_attn_up"])
    sh["w_o"] = f(inp["w_o"])
    lg = np.asarray(inp["ln_g"], np.float32).reshape(2, 2, 8, 128)
    lb = np.asarray(inp["ln_b"], np.float32).reshape(2, 2, 8, 128)
    sh["lng_t"] = np.ascontiguousarray(lg.transpose(3, 0, 1, 2))
    sh["lnb_t"] = np.ascontiguousarray(lb.transpose(3, 0, 1, 2))
    sh["w_ffn_gate"] = f(inp["w_ffn_gate"])
    sh["w_ffn_up"] = f(inp["w_ffn_up"])
    sh["w_ffn_down"] = f(inp["w_ffn_down"])
    wr = np.asarray(inp["w_router"], np.float32)[0].reshape(8, 128, 8)
    sh["w_router_t"] = np.ascontiguousarray(wr.transpose(1, 0, 2))
    sh["w_exp_gate"] = f(inp["w_exp_gate"])
    sh["w_exp_up"] = f(inp["w_exp_up"])
    sh["w_exp_down"] = f(inp["w_exp_down"])
    return sh


def prep_core(inp, pb, sb_, T):
    m = {}
    xp = np.asarray(inp["x_prompt"], np.float32)[pb][:, :T]
    m["xp"] = np.ascontiguousarray(xp.transpose(0, 2, 1)).reshape(len(pb), 8, 128, T)
    xs = np.asarray(inp["x_sample"], np.float32)[sb_].reshape(128, 1024)
    m["xs"] = np.ascontiguousarray(xs.T).reshape(8, 128, 128)
    ck = np.asarray(inp["cache_k"], np.float32)[:, sb_]
    m["ckT"] = np.ascontiguousarray(ck.transpose(0, 1, 3, 4, 2))
    m["cv"] = np.ascontiguousarray(np.asarray(inp["cache_v"], np.float32)[:, sb_])
    sp = np.asarray(inp["state_pool"], np.float32)[:, sb_]
    m["spT"] = np.ascontiguousarray(sp.transpose(0, 1, 3, 2)).reshape(2, 4, 4, 128, 15)
    cp = np.asarray(inp["c_prompt"], np.float32)[pb]
    if len(pb) < 4:
        cp = np.concatenate([cp, np.zeros((4 - len(pb), 1024), np.float32)], 0)
    cs = np.asarray(inp["c_sample"], np.float32)[sb_]
    call = np.concatenate([cp, cs], 0)
    m["cT"] = np.ascontiguousarray(call.T.reshape(8, 128, 8).transpose(1, 0, 2)).reshape(128, 64)
    return m


_NC_CACHE = {}


def get_program(n_seq, T, with_sample):
    key = (n_seq, T, with_sample)
    if key not in _NC_CACHE:
        _NC_CACHE[key] = Builder(n_seq, T, with_sample).build()
    return _NC_CACHE[key]


def kernel(**inputs):
    nc = get_program(4, SEQ, True)
    shared = prep_shared(inputs)
    in_maps = []
    for c in range(NCORES):
        idx = list(range(4 * c, 4 * c + 4))
        m = dict(shared)
        m.update(prep_core(inputs, idx, idx, SEQ))
        in_maps.append(m)
    res = run_bass_kernel_spmd(nc, in_maps, core_ids=list(range(NCORES)))
    r = res.results
    y_prompt = np.concatenate([x["yp"] for x in r], 0)
    y_sample = np.concatenate([x["ys"].reshape(4, 32, D) for x in r], 0)
    nkp = np.concatenate([x["nkp"] for x in r], 1)
    nvp = np.concatenate([x["nvp"] for x in r], 1)
    npp = np.concatenate([x["npp"] for x in r], 1)
    nks = np.concatenate([x["nks"] for x in r], 1)
    nvs = np.concatenate([x["nvs"] for x in r], 1)
    nps = np.concatenate([x["nps"] for x in r], 1)
    return tuple(np.ascontiguousarray(a.astype(np.float32, copy=False))
                 for a in (y_prompt, y_sample, nkp, nvp, npp, nks, nvs, nps))
```
